# Optimizing a Trainium2 kernel written in Bass

```python
import jax
import jax.numpy as jnp
from jax import lax
import numpy as np

D_MODEL = 1024
BATCH = 8
SEQ = 4096
DEPTH = 2

MEM_LEN = 256
BRANCH_W = D_MODEL
NH_M = 4
DV_M = BRANCH_W // NH_M
DQK_M = DV_M // 2
MLSTM_CHUNK = 64
CONV_K = 4
SGU_GROUPS = 4
SGU_CHUNK = 128
D_SGU = BRANCH_W
NH_X = 4
DH_X = BRANCH_W // NH_X
N_BRANCH = 3
DFF = 256 * ((8 * D_MODEL // 3 + 255) // 256)
N_NORMS = 7
EPS = 1e-6
NEG_INIT = -1e30

W_IN_SIZES = (2 * NH_M * DQK_M, NH_M * DV_M, NH_M * DV_M, NH_M, NH_M, D_SGU, D_SGU, NH_X * DH_X, N_BRANCH * BRANCH_W)
W_IN_COLS = sum(W_IN_SIZES)

kernel_name = "hybrid_mlstm_sgu_memxattn_macaron"


def rmsnorm(x, g):
    xf = x.astype(jnp.float32)
    y = xf * lax.rsqrt(jnp.mean(xf * xf, axis=-1, keepdims=True) + EPS)
    return (y * g.astype(jnp.float32)).astype(x.dtype)


def layernorm(x, g, b=None):
    xf = x.astype(jnp.float32)
    mu = jnp.mean(xf, axis=-1, keepdims=True)
    xc = xf - mu
    y = xc * lax.rsqrt(jnp.mean(xc * xc, axis=-1, keepdims=True) + EPS) * g.astype(jnp.float32)
    if b is not None:
        y = y + b.astype(jnp.float32)
    return y.astype(x.dtype)


def swiglu(x, w_gu, w_d):
    a, b = jnp.split(x @ w_gu, 2, axis=-1)
    return (jax.nn.silu(a) * b) @ w_d


def split_cols(z):
    idx = [int(i) for i in np.cumsum(W_IN_SIZES)[:-1]]
    return jnp.split(z, idx, axis=-1)


def causal_dwconv(x, w, b):
    c = x.shape[-1]
    y = lax.conv_general_dilated(x, w[:, None, :].astype(x.dtype), window_strides=(1,),
                                 padding=[(CONV_K - 1, 0)],
                                 dimension_numbers=("NWC", "WIO", "NWC"),
                                 feature_group_count=c)
    return y + b


def mlstm_chunkwise(q, k, v, i_pre, f_pre):
    B_, S_, H, DK = q.shape
    DVv = v.shape[-1]
    NC = S_ // MLSTM_CHUNK
    f32 = jnp.float32

    def to_chunks(t):
        t = t.astype(f32).reshape((B_, NC, MLSTM_CHUNK, H) + t.shape[3:])
        return jnp.moveaxis(t, (1, 3), (0, 2))

    q_c = to_chunks(q)
    k_c = to_chunks(k) * (DK ** -0.5)
    v_c = to_chunks(v)
    i_c = to_chunks(i_pre)
    lf_c = jax.nn.log_sigmoid(to_chunks(f_pre))
    causal = jnp.tril(jnp.ones((MLSTM_CHUNK, MLSTM_CHUNK), dtype=bool))

    def step(carry, xs):
        C, n, m = carry
        qc, kc, vc, ic, fc = xs
        b = jnp.cumsum(fc, axis=-1)
        g = b[..., -1]
        log_d = jnp.where(causal, b[..., :, None] - b[..., None, :] + ic[..., None, :], -jnp.inf)
        m_inter = b + m[..., None]
        m_t = jnp.maximum(m_inter, jnp.max(log_d, axis=-1))
        d = jnp.exp(log_d - m_t[..., None])
        s = jnp.einsum("bhtd,bhsd->bhts", qc, kc) * d
        inter = jnp.exp(m_inter - m_t)
        num = jnp.einsum("bhts,bhsv->bhtv", s, vc) + inter[..., None] * jnp.einsum("bhtd,bhdv->bhtv", qc, C)
        den = jnp.sum(s, axis=-1) + inter * jnp.einsum("bhtd,bhd->bht", qc, n)
        h = num / jnp.maximum(jnp.abs(den), jnp.exp(-m_t))[..., None]
        a = g[..., None] - b + ic
        m_new = jnp.maximum(g + m, jnp.max(a, axis=-1))
        w = jnp.exp(a - m_new[..., None])
        decay = jnp.exp(g + m - m_new)
        C = decay[..., None, None] * C + jnp.einsum("bhs,bhsd,bhsv->bhdv", w, kc, vc)
        n = decay[..., None] * n + jnp.einsum("bhs,bhsd->bhd", w, kc)
        return (C, n, m_new), h

    init = (jnp.zeros((B_, H, DK, DVv), f32), jnp.zeros((B_, H, DK), f32),
            jnp.full((B_, H), NEG_INIT, f32))
    _, h = lax.scan(step, init, (q_c, k_c, v_c, i_c, lf_c))
    h = jnp.moveaxis(h, (0, 2), (1, 3)).reshape(B_, S_, H, DVv)
    return h.astype(v.dtype)


def spatial_gating(u, v, ln_g, ln_b, w_s, b_s):
    B_, S_, _ = u.shape
    nc = S_ // SGU_CHUNK
    dg = D_SGU // SGU_GROUPS
    shp = (B_, nc, SGU_CHUNK, SGU_GROUPS, dg)
    vn = layernorm(v.reshape(shp), ln_g.reshape(SGU_GROUPS, dg), ln_b.reshape(SGU_GROUPS, dg))
    w_causal = jnp.tril(w_s)
    mixed = jnp.einsum("gts,bcsgd->bctgd", w_causal, vn) + b_s.T[:, :, None]
    return (u.reshape(shp) * mixed).reshape(B_, S_, D_SGU)


def memory_cross_attention(xq, mem_n, w_mkv):
    B_, S_, _ = xq.shape
    q = xq.reshape(B_, S_, NH_X, DH_X)
    k, v = jnp.split(mem_n @ w_mkv, 2, axis=-1)
    k = k.reshape(B_, -1, NH_X, DH_X)
    v = v.reshape(B_, -1, NH_X, DH_X)
    s = jnp.einsum("bshd,bmhd->bhsm", q, k).astype(jnp.float32) * (DH_X ** -0.5)
    p = jax.nn.softmax(s, axis=-1).astype(v.dtype)
    return jnp.einsum("bhsm,bmhd->bshd", p, v).reshape(B_, S_, NH_X * DH_X)


def setup_inputs(seed: int = 0) -> dict:
    key = jax.random.key(seed)
    ks = jax.random.split(key, 18)
    nrm = jax.random.normal
    f32 = jnp.float32
    return {
        "x": nrm(ks[0], (BATCH, SEQ, D_MODEL), f32),
        "mem": nrm(ks[1], (BATCH, MEM_LEN, D_MODEL), f32),
        "norm_g": 1.0 + 0.05 * nrm(ks[2], (DEPTH, N_NORMS, D_MODEL), f32),
        "w_ffn_gu": nrm(ks[3], (DEPTH, 2, D_MODEL, 2 * DFF), f32) * D_MODEL ** -0.5,
        "w_ffn_down": nrm(ks[4], (DEPTH, 2, DFF, D_MODEL), f32) * DFF ** -0.5,
        "w_in": nrm(ks[5], (DEPTH, D_MODEL, W_IN_COLS), f32) * D_MODEL ** -0.5,
        "conv_w": nrm(ks[6], (DEPTH, CONV_K, 2 * NH_M * DQK_M), f32) * CONV_K ** -0.5,
        "conv_b": 0.02 * nrm(ks[7], (DEPTH, 2 * NH_M * DQK_M), f32),
        "b_i": 0.1 * nrm(ks[8], (DEPTH, NH_M), f32),
        "b_f": 3.0 + 0.5 * nrm(ks[9], (DEPTH, NH_M), f32),
        "mlstm_norm_g": 1.0 + 0.05 * nrm(ks[10], (DEPTH, NH_M * DV_M), f32),
        "sgu_ln_g": 1.0 + 0.05 * nrm(ks[11], (DEPTH, D_SGU), f32),
        "sgu_ln_b": 0.02 * nrm(ks[12], (DEPTH, D_SGU), f32),
        "sgu_w_s": nrm(ks[13], (DEPTH, SGU_GROUPS, SGU_CHUNK, SGU_CHUNK), f32) * SGU_CHUNK ** -0.5,
        "sgu_b_s": 1.0 + 0.1 * nrm(ks[14], (DEPTH, SGU_GROUPS, SGU_CHUNK), f32),
        "w_mkv": nrm(ks[15], (DEPTH, D_MODEL, 2 * NH_X * DH_X), f32) * D_MODEL ** -0.5,
        "w_branch": nrm(ks[16], (DEPTH, N_BRANCH, BRANCH_W, D_MODEL), f32) * BRANCH_W ** -0.5,
        "w_out": nrm(ks[17], (DEPTH, D_MODEL, D_MODEL), f32) * D_MODEL ** -0.5,
    }


def reference(x, mem, norm_g, w_ffn_gu, w_ffn_down, w_in, conv_w, conv_b, b_i, b_f,
              mlstm_norm_g, sgu_ln_g, sgu_ln_b, sgu_w_s, sgu_b_s, w_mkv, w_branch, w_out):
    h = x
    bs = x.shape[:2]
    for l in range(DEPTH):
        g = norm_g[l]
        f = swiglu(rmsnorm(h, g[0]), w_ffn_gu[l, 0], w_ffn_down[l, 0])
        h = h + 0.5 * rmsnorm(f, g[1])

        xn = rmsnorm(h, g[2])
        z = xn @ w_in[l]
        zqk, zv, zo, zi, zf, zu, zsv, zxq, zg = split_cols(z)

        qk = jax.nn.silu(causal_dwconv(zqk, conv_w[l], conv_b[l]))
        q, k = jnp.split(qk, 2, axis=-1)
        y_m = mlstm_chunkwise(q.reshape(bs + (NH_M, DQK_M)), k.reshape(bs + (NH_M, DQK_M)),
                              zv.reshape(bs + (NH_M, DV_M)), zi + b_i[l], zf + b_f[l])
        y_m = layernorm(y_m, mlstm_norm_g[l].reshape(NH_M, DV_M)).reshape(bs + (NH_M * DV_M,))
        y_m = y_m * jax.nn.sigmoid(zo)

        y_s = spatial_gating(jax.nn.gelu(zu), jax.nn.gelu(zsv), sgu_ln_g[l], sgu_ln_b[l],
                             sgu_w_s[l], sgu_b_s[l])

        y_x = memory_cross_attention(zxq, rmsnorm(mem, g[6]), w_mkv[l])

        gates = jax.nn.sigmoid(zg).reshape(bs + (N_BRANCH, D_MODEL))
        merged = (gates[..., 0, :] * (y_m @ w_branch[l, 0])
                  + gates[..., 1, :] * (y_s @ w_branch[l, 1])
                  + gates[..., 2, :] * (y_x @ w_branch[l, 2]))
        y = merged @ w_out[l]
        h = h + rmsnorm(y, g[3])

        f = swiglu(rmsnorm(h, g[4]), w_ffn_gu[l, 1], w_ffn_down[l, 1])
        h = h + 0.5 * rmsnorm(f, g[5])
    return h
```

```python
import contextlib
import numpy as np
import ml_dtypes
import concourse.bass as bass
import concourse.mybir as mybir
from concourse.bass_utils import run_bass_kernel_spmd

F32 = mybir.dt.float32
BF16 = mybir.dt.bfloat16
AF = mybir.ActivationFunctionType
ALU = mybir.AluOpType
AX = mybir.AxisListType

D = 1024
DFF = 2816
MEM = 256
WIN = 9224
TM = 512
C_QK, C_V, C_O, C_I, C_F, C_U, C_SV, C_XQ, C_G = 0, 1024, 2048, 3072, 3076, 3080, 4104, 5128, 6152
EPS = 1e-6
NSLOT = 3
SLOTW = 4096

ENGINES = ("pe", "act", "dve", "pool", "sp")


class Op:
    __slots__ = ("eng", "fn", "deps", "is_dma", "stream", "signaled", "sem", "val", "clock")

    def __init__(self, eng, fn, is_dma=False, stream=None):
        self.eng = eng
        self.fn = fn
        self.deps = []
        self.is_dma = is_dma
        self.stream = stream
        self.signaled = False
        self.sem = None
        self.val = 0
        self.clock = None


class Prog:
    def __init__(self, nc):
        self.nc = nc
        self.ops = []
        self.by_eng = {e: [] for e in ENGINES}
        self.last_writer = {}
        self.readers = {}

    def _add_dep(self, op, d):
        if d is None or d is op:
            return
        if d.is_dma and op.is_dma and d.stream == op.stream:
            return
        if d not in op.deps:
            op.deps.append(d)

    def op(self, eng, fn, reads=(), writes=(), dma=False, stream=None):
        o = Op(eng, fn, is_dma=dma, stream=stream)
        for t in reads:
            w = self.last_writer.get(t)
            if w is not None and w.eng == eng and eng == "pe" and not w.is_dma and not dma:
                continue
            self._add_dep(o, w)
        for t in writes:
            w = self.last_writer.get(t)
            if w is not None and not (w.eng == eng and not w.is_dma and not dma):
                self._add_dep(o, w)
            for r in self.readers.get(t, ()):
                if not (r.eng == eng and not r.is_dma and not dma):
                    self._add_dep(o, r)
        for t in reads:
            rl = self.readers.setdefault(t, [])
            if not dma:
                for i2 in range(len(rl)):
                    if rl[i2].eng == eng and not rl[i2].is_dma:
                        rl[i2] = o
                        break
                else:
                    rl.append(o)
            else:
                rl.append(o)
        for t in writes:
            self.last_writer[t] = o
            self.readers[t] = []
        self.ops.append(o)
        self.by_eng[eng].append(o)
        return o

    def dma(self, q, out, in_, reads=(), writes=(), stream=None):
        return self.op(q, lambda e: e.dma_start(out=out, in_=in_), reads=reads, writes=writes,
                       dma=True, stream=stream)

    def barrier(self, eng, toks):
        return self.op(eng, None, reads=toks)

    def emit(self):
        nc = self.nc
        for o in self.ops:
            for d in o.deps:
                d.signaled = True
        stack = contextlib.ExitStack()
        eng_sem = {e: stack.enter_context(nc.semaphore("s_" + e)) for e in ENGINES}
        stream_sem = {}
        for o in self.ops:
            if o.is_dma and o.stream not in stream_sem:
                stream_sem[o.stream] = stack.enter_context(nc.semaphore("d_" + o.stream))
        cnt = {e: 0 for e in ENGINES}
        scnt = {s: 0 for s in stream_sem}
        for o in self.ops:
            if o.is_dma:
                scnt[o.stream] += 1
                o.sem = stream_sem[o.stream]
                o.val = 16 * scnt[o.stream]
            elif o.signaled and o.fn is not None:
                cnt[o.eng] += 1
                o.sem = eng_sem[o.eng]
                o.val = cnt[o.eng]
        eng_clock = {e: {} for e in ENGINES}
        waits = {}
        nw = 0
        for o in self.ops:
            ck = eng_clock[o.eng]
            w = []
            for d in o.deps:
                key = id(d.sem)
                if ck.get(key, 0) >= d.val:
                    continue
                w.append((d.sem, d.val))
                for k2, v2 in d.clock.items():
                    if ck.get(k2, 0) < v2:
                        ck[k2] = v2
                ck[key] = d.val
            waits[id(o)] = w
            nw += len(w)
            o.clock = dict(ck) if o.signaled or o.is_dma else None
        self.n_waits = nw
        self.sem_counts = dict(cnt)
        handles = {"pe": "tensor", "act": "scalar", "dve": "vector", "pool": "gpsimd", "sp": "sync"}
        with nc.Block() as block:
            for e in ENGINES:
                ops = self.by_eng[e]
                if not ops:
                    continue

                def body(eh, ops=ops):
                    for o in ops:
                        for (s, v) in waits[id(o)]:
                            eh.wait_ge(s, v)
                        if o.fn is None:
                            continue
                        ins = o.fn(eh)
                        if o.is_dma:
                            ins.then_inc(o.sem, 16)
                        elif o.signaled:
                            ins.then_inc(o.sem, 1)
                getattr(block, handles[e])(body)
        stack.close()


class Cfg:
    def __init__(self, S=4096, L=2, stages="all", preconvert=True):
        self.S = S
        self.L = L
        self.stages = stages
        self.preconvert = preconvert


def build_program(cfg):
    S, L = cfg.S, cfg.L
    NT = S // TM
    nc = bass.Bass("TRN2", target_bir_lowering=False)
    P = Prog(nc)

    def dram_in(name, shape, dt=F32):
        return nc.dram_tensor(name, list(shape), dt, kind="ExternalInput")

    xT_d = dram_in("xT", [D, S])
    memT_d = dram_in("memT", [D, MEM])
    wgu_d = dram_in("w_gu", [L, 2, D, 2 * DFF])
    wdn_d = dram_in("w_dn", [L, 2, DFF, D])
    win_d = dram_in("w_in", [L, D, WIN])
    wkv_d = dram_in("w_kv", [L, D, 2 * D])
    wbr_d = dram_in("w_br", [L, 3, D, D])
    wout_d = dram_in("w_out", [L, D, D])
    NPV = 104 * L
    pv_d = dram_in("pvec", [128, NPV])
    rowp_d = dram_in("rowp", [128, L * 2 * D])
    wsT_d = dram_in("wsT", [128, L * 4 * 128])
    bs_d = dram_in("bs", [1, L * 512])
    bif_d = dram_in("bif", [4, 2 * L])
    ident_d = dram_in("ident", [128, 128], BF16)
    mask_d = dram_in("maskle", [128, 128], BF16)
    sel_d = dram_in("sel", [4, 512])
    yT_d = nc.dram_tensor("yT", [D, S], F32, kind="ExternalOutput")

    if cfg.preconvert:
        sc = lambda name, shape: nc.dram_tensor(name, list(shape), BF16, kind="Internal")
        wgu_b = sc("wgu_b", [L, 2, D, 2 * DFF])
        wdn_b = sc("wdn_b", [L, 2, DFF, D])
        win_b = sc("win_b", [L, D, WIN])
        wkv_b = sc("wkv_b", [L, D, 2 * D])
        wbr_b = sc("wbr_b", [L, 3, D, D])
        wout_b = sc("wout_b", [L, D, D])

    def sb(name, shape, dt):
        return nc.alloc_sbuf_tensor("sb_" + name, list(shape), dt)

    ident = sb("ident", [128, 128], BF16)
    onesb = sb("onesb", [128, 128], BF16)
    maskb = sb("maskb", [128, 128], BF16)
    sel = sb("sel", [4, 512], F32)
    pv = sb("pv", [128, NPV], F32)
    pvh = sb("pvh", [128, NPV], F32)
    rowp = sb("rowp", [128, L * 2 * D], BF16)
    wsT = sb("wsT", [128, L * 4 * 128], BF16)
    bshi = sb("bshi", [1, L * 512], BF16)
    bslo = sb("bslo", [1, L * 512], BF16)
    bif = sb("bif", [4, 2 * L], F32)
    ones4 = sb("ones4", [4, 512], F32)
    Cst = sb("Cst", [128, L * 4 * 257], F32)
    tails = sb("tails", [128, L * 8 * 3], F32)
    Bprev = sb("Bprev", [4, L], F32)
    MU = sb("MU", [4, L * 5], F32)
    kmT = sb("kmT", [128, L * 8 * 256], BF16)
    vm = sb("vm", [128, L * 2 * 1024], BF16)
    h = sb("h", [128, 8, TM], F32)
    xn = sb("xn", [128, 8, TM], BF16)
    sq = sb("sq", [128, 4, TM], BF16)
    rstd = sb("rstd", [128, TM], F32)
    wslot = [sb("wslot%d" % i, [128, SLOTW], BF16) for i in range(NSLOT)]
    a012 = sb("a012", [128, 12288], BF16)
    bufA = a012[:, 0:4096].rearrange("p (k t) -> p k t", k=8)
    bufB = a012[:, 4096:8192].rearrange("p (k t) -> p k t", k=8)
    bufC = a012[:, 8192:12288].rearrange("p (k t) -> p k t", k=8)
    hid = a012[:, 0:22 * TM].rearrange("p (k t) -> p k t", k=22)
    ymT_t = sb("ymT", [128, 8 * TM], BF16)
    ymT = ymT_t[:, :].rearrange("p (k t) -> p k t", k=8)
    sil = ymT_t[:, 0:4 * TM].bitcast(F32).rearrange("p (k t) -> p k t", k=2)
    xqT = sb("xqT", [128, 8, TM], BF16)
    a5 = sb("a5", [128, 10320], BF16)
    vn = a5[:, 0:4096].rearrange("p (c n) -> p c n", c=4)
    kp = a5[:, 4096:6144].rearrange("p (c n) -> p c n", c=4)
    vaug = a5[:, 6144:6144 + 4112].rearrange("p (c h n) -> p c h n", c=4, h=4)
    fT = a5[:, 0:8192].bitcast(F32).rearrange("p (k t) -> p k t", k=8)
    gv = sb("gv", [128, 2, 1024], F32)
    zb = sb("zb", [128, 2, TM + 3], F32)
    ctmp = sb("ctmp", [128, 2, TM], F32)
    mtmp = ctmp
    sig = sb("sig", [128, 2, TM], BF16)
    Cbf = sb("Cbf", [128, 4, 4 * 257], BF16)
    ST = sb("ST", [128, 4, 512], BF16)
    hn = sb("hn", [128, 2, 1024], BF16)
    pb = hn
    pT = sb("pT", [128, 2, 1024], BF16)
    gI = sb("gI", [4, TM], F32)
    gF = gv[0:4, 0, 0:TM]
    gT1 = gv[0:4, 0, TM:2 * TM]
    gT2 = gv[0:4, 1, 0:TM]
    gB = gv[0:4, 1, TM:2 * TM]
    gW = sb("gW", [4, TM], F32)
    gE = sb("gE", [4, TM], F32)
    mx4 = sb("mx4", [4, 4], F32)
    dec4 = sb("dec4", [4, 4], F32)
    decb = sb("decb", [128, 16], F32)
    tokW = sb("tokW", [128, 4, 8], F32)
    st6 = sb("st6", [128, 4, 6], F32)
    st8 = sb("st8", [128, 8, 6], F32)
    mv8 = sb("mv8", [128, 8, 2], F32)
    mv = sb("mv", [128, 4, 2], F32)
    sm = sb("sm", [128, 2, 16], F32)
    dummy = sb("dummy", [1, 8], F32)
    epsb = sb("epsb", [128, 1], F32)

    banks = [nc.alloc_psum_tensor("bank%d" % i, [128, 512], F32) for i in range(8)]
    bank_rr = [0]

    def nextbank(lo=0, hi=6):
        i = lo + bank_rr[0] % (hi - lo)
        bank_rr[0] += 1
        return banks[i], ("bank", i)

    def T(name, k=None, c=None):
        if k is None:
            return [(name,)]
        if c is None:
            return [(name, k)]
        return [(name, k, c)]

    ARENA_TOKS = []

    def reg_arena(toks):
        ARENA_TOKS.extend(toks)
        return toks

    for k in range(8):
        reg_arena([("bufA", k), ("bufB", k), ("bufC", k), ("ymT", k), ("fT", k)])
    for j in range(22):
        reg_arena([("hid", j)])
    reg_arena([("sil", 0), ("sil", 1)])
    for c in range(4):
        reg_arena([("vn", c), ("kp", c), ("vaug", c)])

    def fence():
        P.op("pool", lambda e: e.memset(dummy[:, 0:1], 0.0), writes=list(ARENA_TOKS))

    def pvcol(kind, l, a=0, b=0):
        if kind == "ng":
            return (l * 7 + a) * 8 + b
        if kind == "cw":
            return L * 56 + (l * 4 + a) * 8 + b
        if kind == "cb":
            return L * 88 + l * 8 + a
        if kind == "mg":
            return L * 96 + l * 8 + a
        raise ValueError

    P.dma("sp", ident[:], ident_d.ap(), writes=T("ident"), stream="c0")
    P.dma("sp", maskb[:], mask_d.ap(), writes=T("maskb"), stream="c2")
    P.dma("sp", sel[:], sel_d.ap(), writes=T("sel"), stream="c3")
    P.dma("sp", pv[:], pv_d.ap(), writes=T("pv"), stream="c4")
    P.dma("sp", bif[:], bif_d.ap(), writes=T("bif"), stream="c5")
    P.dma("pool", rowp[:], rowp_d.ap(), writes=T("rowp"), stream="c1")
    gvflat = gv[:, :, :].rearrange("p a b -> p (a b)")
    bsf = gv[0:1, 1, 0:L * 512]
    P.dma("sp", bsf, bs_d.ap(), writes=T("bsf") + T("gv", 1), stream="c6")
    P.dma("sp", gvflat[:, 0:L * 512], wsT_d.ap(), writes=T("gv", 0), stream="c7")
    for l in range(L):
        for g in range(4):
            o0 = (l * 4 + g) * 128
            P.op("dve", lambda e, o0=o0: e.tensor_tensor(out=wsT[:, o0:o0 + 128], in0=gvflat[:, o0:o0 + 128],
                                                         in1=maskb[:], op=ALU.mult),
                 reads=T("gv", 0) + T("maskb"), writes=T("wsT"))
    P.op("dve", lambda e: e.memset(onesb[:], 1.0), writes=T("onesb"))
    P.op("dve", lambda e: e.memset(epsb[:], EPS), writes=T("epsb"))
    P.op("dve", lambda e: e.memset(ones4[:], 1.0), writes=T("ones4"))
    P.op("dve", lambda e: e.memset(Cst[:], 0.0), writes=[("C", l) for l in range(L)])
    P.op("dve", lambda e: e.memset(tails[:], 0.0), writes=[("tails", l) for l in range(L)])
    P.op("dve", lambda e: e.memset(Bprev[:], 0.0), writes=[("Bprev", l) for l in range(L)])
    P.op("dve", lambda e: e.memset(MU[:], -1e30), writes=[("MU", l) for l in range(L)])
    P.op("dve", lambda e: e.tensor_scalar(out=pvh[:], in0=pv[:], scalar1=0.5, scalar2=None, op0=ALU.mult),
         reads=T("pv"), writes=T("pvh"))
    P.op("dve", lambda e: e.tensor_copy(out=bshi[:], in_=bsf), reads=T("bsf"), writes=T("bshi"))
    P.op("dve", lambda e: e.tensor_tensor(out=bslo[:], in0=bsf, in1=bshi[:], op=ALU.subtract),
         reads=T("bsf") + T("bshi"), writes=T("bslo") + T("gv", 1))

    jobs = []
    job_last = {}
    jp = [0]

    def cvt(dst, src, tok, stream):
        job_last[tok] = len(jobs)
        jobs.append((dst, src, tok, stream))

    def pump(n):
        while n > 0 and jp[0] < len(jobs):
            dst, src, tok, stream = jobs[jp[0]]
            P.dma("pool", dst, src, writes=[tok], stream=stream)
            jp[0] += 1
            n -= 1

    def ensure(tok):
        if tok in job_last:
            while jp[0] <= job_last[tok]:
                pump(1)

    if cfg.preconvert:
        for l in range(L):
            for k in range(8):
                cvt(wkv_b.ap()[l, k * 128:(k + 1) * 128, :], wkv_d.ap()[l, k * 128:(k + 1) * 128, :],
                    ("cv", l, "kv"), "cv%dkv" % l)
        for l in range(L):
            for f in range(2):
                if f == 1:
                    for k in range(8):
                        for hf in range(4):
                            c0 = hf * 2306
                            cvt(win_b.ap()[l, k * 128:(k + 1) * 128, c0:c0 + 2306],
                                win_d.ap()[l, k * 128:(k + 1) * 128, c0:c0 + 2306], ("cv", l, "in"), "cv%din" % l)
                    for b in range(3):
                        for k in range(8):
                            cvt(wbr_b.ap()[l, b, k * 128:(k + 1) * 128, :], wbr_d.ap()[l, b, k * 128:(k + 1) * 128, :],
                                ("cv", l, "br"), "cv%dbr" % l)
                    for k in range(8):
                        cvt(wout_b.ap()[l, k * 128:(k + 1) * 128, :], wout_d.ap()[l, k * 128:(k + 1) * 128, :],
                            ("cv", l, "out"), "cv%dout" % l)
                for k in range(8):
                    for hf in range(2):
                        cvt(wgu_b.ap()[l, f, k * 128:(k + 1) * 128, hf * DFF:(hf + 1) * DFF],
                            wgu_d.ap()[l, f, k * 128:(k + 1) * 128, hf * DFF:(hf + 1) * DFF],
                            ("cv", l, "gu", f), "cv%dgu%d" % (l, f))
                for j in range(22):
                    cvt(wdn_b.ap()[l, f, j * 128:(j + 1) * 128, :], wdn_d.ap()[l, f, j * 128:(j + 1) * 128, :],
                        ("cv", l, "dn", f), "cv%ddn%d" % (l, f))
        WGU, WDN, WINB, WKV, WBR, WOUT = wgu_b, wdn_b, win_b, wkv_b, wbr_b, wout_b
        wq = "sp"
    else:
        WGU, WDN, WINB, WKV, WBR, WOUT = wgu_d, wdn_d, win_d, wkv_d, wbr_d, wout_d
        wq = "pool"

    def cvtok(l, kind, f=None):
        if not cfg.preconvert:
            return []
        return [("cv", l, kind)] if f is None else [("cv", l, kind, f)]

    slot_rr = [0]

    def load_slot(parts, rtoks):
        i = slot_rr[0] % NSLOT
        slot_rr[0] += 1
        st = wslot[i]
        for tk in rtoks:
            ensure(tk)
        pump(3)
        for (dstf, src) in parts:
            P.dma(wq, dstf(st), src, reads=rtoks, writes=[("w", i)], stream="w%d" % i)
        return st, ("w", i)

    def kview(st, ncols, off=0):
        return st[:, off:off + 8 * ncols].rearrange("p (k c) -> p k c", k=8)

    def load_kcols(W_ap2d, c0, ncols, rtoks, extra=None, extra_W=None):
        parts = [(lambda st: kview(st, ncols), W_ap2d[:, c0:c0 + ncols].rearrange("(k p) c -> p k c", p=128))]
        if extra is not None:
            c1, n1 = extra
            W2 = W_ap2d if extra_W is None else extra_W
            parts.append((lambda st: kview(st, n1, off=8 * ncols),
                          W2[:, c1:c1 + n1].rearrange("(k p) c -> p k c", p=128)))
        return load_slot(parts, rtoks)

    def mm(out, lhsT, rhs, start, stop, reads, wtok):
        P.op("pe", lambda e: e.matmul(out, lhsT=lhsT, rhs=rhs, start=start, stop=stop), reads=reads, writes=[wtok])

    def rms_stats(src_fn, src_toks_fn, ncols=TM, scale=1.0 / D):
        bk, btok = banks[7], ("bank", 7)
        for k in range(8):
            if k % 2 == 0:
                P.op("act", lambda e, k=k: e.activation(out=sq[:, k % 4, 0:ncols], in_=src_fn(k), func=AF.Square),
                     reads=src_toks_fn(k), writes=T("sq", k % 4))
            else:
                P.op("dve", lambda e, k=k: e.tensor_tensor(out=sq[:, k % 4, 0:ncols], in0=src_fn(k), in1=src_fn(k),
                                                           op=ALU.mult),
                     reads=src_toks_fn(k), writes=T("sq", k % 4))
            mm(bk[:, 0:ncols], onesb[:], sq[:, k % 4, 0:ncols], k == 0, k == 7, T("sq", k % 4) + T("onesb"), btok)
        P.op("act", lambda e: e.activation(out=rstd[:, 0:ncols], in_=bk[:, 0:ncols], func=AF.Ln, bias=epsb[:, 0:1], scale=scale),
             reads=[btok] + T("epsb"), writes=T("rstd"))
        P.op("act", lambda e: e.activation(out=rstd[:, 0:ncols], in_=rstd[:, 0:ncols], func=AF.Exp, scale=-0.5),
             reads=T("rstd"), writes=T("rstd"))

    def prenorm(l, n):
        rms_stats(lambda k: h[:, k, :], lambda k: T("h", k))
        for k in range(8):
            col = pvcol("ng", l, n, k)
            P.op("dve", lambda e, k=k, col=col: e.scalar_tensor_tensor(
                out=xn[:, k, :], in0=h[:, k, :], scalar=pv[:, col:col + 1], in1=rstd[:, :], op0=ALU.mult, op1=ALU.mult),
                reads=T("h", k) + T("rstd") + T("pv"), writes=T("xn", k))

    def postnorm_residual(l, n, half):
        rms_stats(lambda k: fT[:, k, :], lambda k: T("fT", k))
        pvt = pvh if half else pv
        ptok = T("pvh") if half else T("pv")
        for k in range(8):
            col = pvcol("ng", l, n, k)
            P.op("dve", lambda e, k=k, col=col: e.scalar_tensor_tensor(
                out=fT[:, k, :], in0=fT[:, k, :], scalar=pvt[:, col:col + 1], in1=rstd[:, :], op0=ALU.mult, op1=ALU.mult),
                reads=T("fT", k) + T("rstd") + ptok, writes=T("fT", k))
            P.op("dve", lambda e, k=k: e.tensor_tensor(out=h[:, k, :], in0=h[:, k, :], in1=fT[:, k, :], op=ALU.add),
                 reads=T("fT", k) + T("h", k), writes=T("h", k))

    def ffn(l, f):
        prenorm(l, 0 if f == 0 else 4)
        Wgu = WGU.ap()[l, f]
        Wdn = WDN.ap()[l, f]
        for grp in range(11):
            st, wt = load_kcols(Wgu, grp * 256, 256, cvtok(l, "gu", f), extra=(DFF + grp * 256, 256))
            wa = kview(st, 256)
            wb = kview(st, 256, off=2048)
            for jj in range(2):
                j = grp * 2 + jj
                pa, ta = nextbank()
                pbk, tb = nextbank()
                for k in range(8):
                    mm(pa[:, :], wa[:, k, jj * 128:(jj + 1) * 128], xn[:, k, :], k == 0, k == 7, [wt] + T("xn", k), ta)
                for k in range(8):
                    mm(pbk[:, :], wb[:, k, jj * 128:(jj + 1) * 128], xn[:, k, :], k == 0, k == 7, [wt] + T("xn", k), tb)
                P.op("act", lambda e, pa=pa, j=j: e.activation(out=sil[:, j % 2, :], in_=pa[:, :], func=AF.Silu),
                     reads=[ta], writes=T("sil", j % 2))
                P.op("dve", lambda e, pbk=pbk, j=j: e.tensor_tensor(out=hid[:, j, :], in0=sil[:, j % 2, :], in1=pbk[:, :],
                                                                  op=ALU.mult),
                     reads=[tb] + T("sil", j % 2), writes=T("hid", j))
        for mp in range(4):
            pm = [nextbank(), nextbank()]
            for half in range(2):
                parts = [(lambda st: st[:, 0:11 * 256].rearrange("p (j c) -> p j c", j=11),
                          Wdn[half * 1408:(half + 1) * 1408, mp * 256:(mp + 1) * 256].rearrange("(j p) c -> p j c", p=128))]
                st, wt = load_slot(parts, cvtok(l, "dn", f))
                wv = st[:, 0:11 * 256].rearrange("p (j c) -> p j c", j=11)
                for mi in range(2):
                    pk, tk = pm[mi]
                    for jj in range(11):
                        j = half * 11 + jj
                        mm(pk[:, :], wv[:, jj, mi * 128:(mi + 1) * 128], hid[:, j, :], j == 0, j == 21,
                           [wt] + T("hid", j), tk)
            for mi in range(2):
                m = mp * 2 + mi
                pk, tk = pm[mi]
                P.op("act", lambda e, pk=pk, m=m: e.activation(out=fT[:, m, :], in_=pk[:, :], func=AF.Copy),
                     reads=[tk], writes=T("fT", m))
        postnorm_residual(l, 1 if f == 0 else 5, True)

    def setup_kv():
        mt = h
        for k in range(8):
            P.dma("sp", h[:, k, 0:MEM], memT_d.ap()[k * 128:(k + 1) * 128, :], writes=T("h", k), stream="ld_h%d" % k)
        for l in range(L):
            rms_stats(lambda k: h[:, k, 0:MEM], lambda k: T("h", k), ncols=MEM)
            for k in range(8):
                col = pvcol("ng", l, 6, k)
                P.op("dve", lambda e, k=k, col=col: e.scalar_tensor_tensor(
                    out=xn[:, k, 0:MEM], in0=h[:, k, 0:MEM], scalar=pv[:, col:col + 1], in1=rstd[:, 0:MEM],
                    op0=ALU.mult, op1=ALU.mult), reads=T("h", k) + T("rstd") + T("pv"), writes=T("xn", k))
            Wkv = WKV.ap()[l]
            for g in range(4):
                st, wt = load_kcols(Wkv, g * 512, 512, cvtok(l, "kv"))
                wv = kview(st, 512)
                if g < 2:
                    for jj in range(4):
                        j = g * 4 + jj
                        pk, tk = nextbank()
                        for k in range(8):
                            mm(pk[:, 0:MEM], wv[:, k, jj * 128:(jj + 1) * 128], xn[:, k, 0:MEM], k == 0, k == 7,
                               [wt] + T("xn", k), tk)
                        o0 = (l * 8 + j) * 256
                        P.op("act", lambda e, pk=pk, o0=o0: e.activation(out=kmT[:, o0:o0 + 256], in_=pk[:, 0:MEM], func=AF.Copy),
                             reads=[tk], writes=T("kmT", l))
                else:
                    for mc in range(2):
                        pk, tk = nextbank()
                        for k in range(8):
                            mm(pk[:, :], xn[:, k, mc * 128:(mc + 1) * 128], wv[:, k, :], k == 0, k == 7,
                               [wt] + T("xn", k), tk)
                        o0 = (l * 2 + mc) * 1024 + (g - 2) * 512
                        P.op("act", lambda e, pk=pk, o0=o0: e.activation(out=vm[:, o0:o0 + 512], in_=pk[:, :], func=AF.Copy),
                             reads=[tk], writes=T("vm", l))

    def mixer(l):
        prenorm(l, 2)
        Win = WINB.ap()[l]
        Ct = Cst[:, l * 1028:(l + 1) * 1028].rearrange("p (h n) -> p h n", h=4)
        stg, wtg = load_kcols(Win, C_I, 8, cvtok(l, "in"))
        wg8 = kview(stg, 8)
        bk6, t6 = banks[6], ("bank", 6)
        for k in range(8):
            mm(bk6[0:4, 0:TM], wg8[:, k, 0:4], xn[:, k, :], k == 0, k == 7, [wtg] + T("xn", k), t6)
        bk7, t7 = banks[7], ("bank", 7)
        for k in range(8):
            mm(bk7[0:4, 0:TM], wg8[:, k, 4:8], xn[:, k, :], k == 0, k == 7, [wtg] + T("xn", k), t7)
        bi = bif[:, 2 * l:2 * l + 1]
        bfp = bif[:, 2 * l + 1:2 * l + 2]
        dve = lambda fn, r, w: P.op("dve", fn, reads=r, writes=w)
        act = lambda fn, r, w: P.op("act", fn, reads=r, writes=w)
        dve(lambda e: e.tensor_scalar(out=gF, in0=bk7[0:4, 0:TM], scalar1=bfp, scalar2=None, op0=ALU.add),
            [t7] + T("bif"), T("gv", 0))
        act(lambda e: e.activation(out=gT1, in_=gF, func=AF.Abs), T("gv", 0), T("gv", 0))
        act(lambda e: e.activation(out=gT2, in_=gT1, func=AF.Exp, scale=-1.0), T("gv", 0), T("gv", 1))
        act(lambda e: e.activation(out=gT2, in_=gT2, func=AF.Ln, bias=1.0), T("gv", 1), T("gv", 1))
        dve(lambda e: e.tensor_single_scalar(out=gT1, in_=gF, scalar=0.0, op=ALU.min), T("gv", 0), T("gv", 0))
        dve(lambda e: e.tensor_tensor(out=gF, in0=gT1, in1=gT2, op=ALU.subtract), T("gv", 0) + T("gv", 1), T("gv", 0))
        dve(lambda e: e.tensor_tensor_scan(out=gB, data0=ones4[:], data1=gF, initial=Bprev[:, l:l + 1],
                                           op0=ALU.mult, op1=ALU.add), T("gv", 0) + T("ones4") + [("Bprev", l)], T("gv", 1))
        dve(lambda e: e.tensor_copy(out=Bprev[:, l:l + 1], in_=gB[:, TM - 1:TM]), T("gv", 1), [("Bprev", l)])
        dve(lambda e: e.scalar_tensor_tensor(out=gI[:], in0=bk6[0:4, 0:TM], scalar=bi, in1=gB, op0=ALU.add,
                                             op1=ALU.subtract), [t6] + T("bif") + T("gv", 1), T("gI"))
        dve(lambda e: e.tensor_reduce(out=mx4[:], in_=gI[:, :].rearrange("p (c t) -> p c t", c=4), axis=AX.X, op=ALU.max),
            T("gI"), T("mx4"))
        mul = MU[:, l * 5:(l + 1) * 5]
        dve(lambda e: e.tensor_tensor_scan(out=mul[:, 1:5], data0=ones4[:, 0:4], data1=mx4[:], initial=mul[:, 0:1],
                                           op0=ALU.mult, op1=ALU.max), T("mx4") + T("ones4") + [("MU", l)], [("MU", l)])
        dve(lambda e: e.tensor_tensor(out=dec4[:], in0=mul[:, 0:4], in1=mul[:, 1:5], op=ALU.subtract), [("MU", l)], T("dec4"))
        act(lambda e: e.activation(out=dec4[:], in_=dec4[:], func=AF.Exp), T("dec4"), T("dec4"))
        mub = mul[:, 1:5].unsqueeze(2).to_broadcast([4, 4, 128])
        v3 = lambda t: t[:, :].rearrange("p (c t) -> p c t", c=4)
        dve(lambda e: e.tensor_tensor(out=v3(gT1), in0=v3(gI), in1=mub, op=ALU.subtract), T("gI") + [("MU", l)], T("gv", 0))
        dve(lambda e: e.tensor_scalar(out=gT1, in0=gT1, scalar1=-0.5 * float(np.log(128.0)), scalar2=None, op0=ALU.add),
            T("gv", 0), T("gv", 0))
        act(lambda e: e.activation(out=gW[:], in_=gT1, func=AF.Exp), T("gv", 0), T("gW"))
        dve(lambda e: e.tensor_tensor(out=v3(gT2), in0=v3(gB), in1=mub, op=ALU.add), T("gv", 1) + [("MU", l)], T("gv", 1))
        act(lambda e: e.activation(out=gE[:], in_=gT2, func=AF.Exp, scale=-2.0), T("gv", 1), T("gE"))
        dve(lambda e: e.tensor_copy(out=mul[:, 0:1], in_=mul[:, 4:5]), [("MU", l)] + T("dec4") + T("gv", 0) + T("gv", 1), [("MU", l)])
        P.op("pool", lambda e: e.memset(vaug[:, :, :, 256:257], 1.0), writes=[("vaug", c) for c in range(4)])
        tl = tails[:, l * 24:(l + 1) * 24].rearrange("p (b t) -> p b t", b=8)
        for g in range(2):
            st, wt = load_kcols(Win, C_QK + g * 512, 512, cvtok(l, "in"))
            wv = kview(st, 512)
            for jj in range(4):
                blk = g * 4 + jj
                zi = blk % 2
                pk, tk = nextbank()
                for k in range(8):
                    mm(pk[:, :], wv[:, k, jj * 128:(jj + 1) * 128], xn[:, k, :], k == 0, k == 7, [wt] + T("xn", k), tk)
                P.op("pool", lambda e, blk=blk, zi=zi: e.tensor_copy(out=zb[:, zi, 0:3], in_=tl[:, blk, :]),
                     reads=[("tails", l)], writes=T("zb", zi))
                act(lambda e, pk=pk, zi=zi: e.activation(out=zb[:, zi, 3:TM + 3], in_=pk[:, :], func=AF.Copy),
                    [tk], T("zb", zi))
                P.op("pool", lambda e, blk=blk, zi=zi: e.tensor_copy(out=tl[:, blk, :], in_=zb[:, zi, TM:TM + 3]),
                     reads=T("zb", zi), writes=[("tails", l)])
                for j in range(4):
                    col = pvcol("cw", l, j, blk)
                    if j == 0:
                        P.op("dve", lambda e, zi=zi, col=col: e.tensor_scalar(
                            out=ctmp[:, zi, :], in0=zb[:, zi, 0:TM], scalar1=pv[:, col:col + 1], scalar2=None, op0=ALU.mult),
                            reads=T("zb", zi) + T("pv"), writes=T("ctmp", zi))
                    else:
                        P.op("dve", lambda e, zi=zi, col=col, j=j: e.scalar_tensor_tensor(
                            out=ctmp[:, zi, :], in0=zb[:, zi, j:TM + j], scalar=pv[:, col:col + 1], in1=ctmp[:, zi, :],
                            op0=ALU.mult, op1=ALU.add),
                            reads=T("zb", zi) + T("pv") + T("ctmp", zi), writes=T("ctmp", zi))
                colb = pvcol("cb", l, blk)
                act(lambda e, zi=zi, blk=blk, colb=colb: e.activation(out=bufA[:, blk, :], in_=ctmp[:, zi, :], func=AF.Silu,
                                                                       bias=pv[:, colb:colb + 1]),
                    T("ctmp", zi) + T("pv"), T("bufA", blk))
        for g in range(2):
            st, wt = load_kcols(Win, C_V + g * 512, 512, cvtok(l, "in"))
            wv = kview(st, 512)
            for c in range(4):
                pk, tk = nextbank()
                for k in range(8):
                    mm(pk[:, :], xn[:, k, c * 128:(c + 1) * 128], wv[:, k, :], k == 0, k == 7, [wt] + T("xn", k), tk)
                act(lambda e, pk=pk, c=c, g=g: e.activation(
                    out=vaug[:, c, 2 * g:2 * g + 2, 0:256], in_=pk[:, :].rearrange("p (h n) -> p h n", h=2), func=AF.Copy),
                    [tk], T("vaug", c))
        for g in range(2):
            st, wt = load_kcols(Win, C_O + g * 512, 512, cvtok(l, "in"))
            wv = kview(st, 512)
            for jj in range(4):
                blk = g * 4 + jj
                pk, tk = nextbank()
                for k in range(8):
                    mm(pk[:, :], wv[:, k, jj * 128:(jj + 1) * 128], xn[:, k, :], k == 0, k == 7, [wt] + T("xn", k), tk)
                act(lambda e, pk=pk, blk=blk: e.activation(out=bufB[:, blk, :], in_=pk[:, :], func=AF.Sigmoid),
                    [tk], T("bufB", blk))
                col = pvcol("mg", l, blk)
                P.op("dve", lambda e, blk=blk, col=col: e.tensor_scalar(
                    out=bufB[:, blk, :], in0=bufB[:, blk, :], scalar1=pv[:, col:col + 1], scalar2=None, op0=ALU.mult),
                    reads=T("bufB", blk) + T("pv"), writes=T("bufB", blk))
        for c in range(4):
            mm(bk6[:, c * 8:c * 8 + 4], gW[:, c * 128:(c + 1) * 128], sel[:, :].rearrange("p (h m) -> p h m", h=4)[:, :, 0],
               True, True, T("gW") + T("sel"), t6)
            mm(bk6[:, c * 8 + 4:c * 8 + 8], gE[:, c * 128:(c + 1) * 128], sel[:, :].rearrange("p (h m) -> p h m", h=4)[:, :, 0],
               True, True, T("gE") + T("sel"), t6)
        dve(lambda e: e.tensor_copy(out=tokW[:, :, :].rearrange("p c n -> p (c n)"), in_=bk6[:, 0:32]), [t6], T("tokW"))
        for hh in range(4):
            mm(bk7[:, hh * 4:hh * 4 + 4], sel[:, hh * 128:(hh + 1) * 128], dec4[:, :], True, True, T("dec4") + T("sel"), t7)
        dve(lambda e: e.tensor_copy(out=decb[:, :], in_=bk7[:, 0:16]), [t7], T("decb"))

        for c in range(4):
            pk, tk = nextbank()
            pkb = pk[:, :].bitcast(BF16)
            for hh in range(4):
                P.op("pe", lambda e, pkb=pkb, hh=hh, c=c: e.transpose(out=pkb[:, hh * 128:(hh + 1) * 128],
                                                                      in_=bufA[:, 4 + hh, c * 128:(c + 1) * 128],
                                                                      identity=ident[:]),
                     reads=T("bufA", 4 + hh) + T("ident"), writes=[tk])
            dve(lambda e, pkb=pkb, c=c: e.tensor_tensor(
                out=kp[:, c, :].rearrange("p (h d) -> p h d", h=4), in0=pkb[:, 0:512].rearrange("p (h d) -> p h d", h=4),
                in1=tokW[:, c, 0:4].unsqueeze(2).to_broadcast([128, 4, 128]), op=ALU.mult),
                [tk] + T("tokW"), T("kp", c))
        fill_rr = [0]

        def filler(i):
            which, g = ("u", i) if i < 2 else ("xq", i - 2)
            c0 = (C_U if which == "u" else C_XQ) + g * 512
            st, wt = load_kcols(Win, c0, 512, cvtok(l, "in"))
            wv = kview(st, 512)
            for jj in range(4):
                blk = g * 4 + jj
                bi_ = 6 + fill_rr[0] % 2
                fill_rr[0] += 1
                pk, tk = banks[bi_], ("bank", bi_)
                for k in range(8):
                    mm(pk[:, :], wv[:, k, jj * 128:(jj + 1) * 128], xn[:, k, :], k == 0, k == 7, [wt] + T("xn", k), tk)
                if which == "u":
                    act(lambda e, pk=pk, blk=blk: e.activation(out=bufC[:, blk, :], in_=pk[:, :], func=AF.Gelu_apprx_tanh),
                        [tk], T("bufC", blk))
                else:
                    act(lambda e, pk=pk, blk=blk: e.activation(out=xqT[:, blk, :], in_=pk[:, :], func=AF.Copy),
                        [tk], T("xqT", blk))

        for c in range(4):
            cs = slice(c * 128, (c + 1) * 128)
            pS, tS = nextbank()
            for hh in range(4):
                mm(pS[:, hh * 128:(hh + 1) * 128], bufA[:, 4 + hh, cs], bufA[:, hh, cs], True, True,
                   T("bufA", 4 + hh) + T("bufA", hh), tS)
            for hh in range(4):
                dve(lambda e, hh=hh, c=c, pS=pS: e.scalar_tensor_tensor(
                    out=ST[:, c, hh * 128:(hh + 1) * 128], in0=pS[:, hh * 128:(hh + 1) * 128], scalar=tokW[:, c, hh:hh + 1],
                    in1=maskb[:], op0=ALU.mult, op1=ALU.mult), [tS] + T("tokW") + T("maskb"), T("ST", c))
            for hh in range(4):
                act(lambda e, hh=hh, c=c: e.activation(out=Cbf[:, c, hh * 257:(hh + 1) * 257], in_=Ct[:, hh, :],
                                                       func=AF.Copy, scale=decb[:, hh * 4 + c:hh * 4 + c + 1]),
                    [("C", l)] + T("decb"), T("Cbf", c))
            for hh in range(4):
                pP, tP = nextbank()
                mm(pP[:, 0:257], kp[:, c, hh * 128:(hh + 1) * 128], vaug[:, c, hh, :], True, True,
                   T("kp", c) + T("vaug", c), tP)
                dve(lambda e, hh=hh, c=c, pP=pP: e.scalar_tensor_tensor(
                    out=Ct[:, hh, :], in0=Ct[:, hh, :], scalar=decb[:, hh * 4 + c:hh * 4 + c + 1], in1=pP[:, 0:257],
                    op0=ALU.mult, op1=ALU.add), [tP, ("C", l)] + T("decb"), [("C", l)])
        for c in range(4):
            cs = slice(c * 128, (c + 1) * 128)
            ci = c % 2
            si = c % 2
            smv = sm[:, si, :]
            pNs = []
            for hh in range(4):
                pN, tN = nextbank()
                pNs.append((pN, tN))
                mm(pN[:, 0:257], ST[:, c, hh * 128:(hh + 1) * 128], vaug[:, c, hh, :], True, False,
                   T("ST", c) + T("vaug", c), tN)
                mm(pN[:, 0:257], bufA[:, hh, cs], Cbf[:, c, hh * 257:(hh + 1) * 257], False, True,
                   T("bufA", hh) + T("Cbf", c), tN)
            pT8, tT8 = nextbank()
            pT8b = pT8[:, :].bitcast(BF16)
            filler(c)
            for hh in range(4):
                pN, tN = pNs[hh]
                dve(lambda e, pN=pN, hh=hh: e.bn_stats(out=st6[:, hh, :], in_=pN[:, 0:256]), [tN], T("st6", hh))
                dve(lambda e, pN=pN, hh=hh, smv=smv: e.tensor_copy(out=smv[:, hh:hh + 1], in_=pN[:, 256:257]),
                    [tN], T("sm", si))
            for hh in range(4):
                dve(lambda e, hh=hh: e.bn_aggr(out=mv[:, hh, :], in_=st6[:, hh, :]), T("st6", hh), T("mv", hh))
            dve(lambda e, smv=smv: e.tensor_tensor(out=smv[:, 4:8], in0=smv[:, 0:4], in1=smv[:, 0:4], op=ALU.mult),
                T("sm", si), T("sm", si))
            dve(lambda e, smv=smv, c=c: e.tensor_tensor(out=smv[:, 4:8], in0=smv[:, 4:8], in1=tokW[:, c, 4:8], op=ALU.max),
                T("sm", si) + T("tokW"), T("sm", si))
            dve(lambda e, smv=smv: e.scalar_tensor_tensor(out=smv[:, 8:12], in0=smv[:, 4:8], scalar=EPS, in1=mv[:, :, 1],
                                                          op0=ALU.mult, op1=ALU.add),
                T("sm", si) + [("mv", g4) for g4 in range(4)], T("sm", si))
            act(lambda e, smv=smv: e.activation(out=smv[:, 12:16], in_=smv[:, 8:12], func=AF.Sqrt), T("sm", si), T("sm", si))
            dve(lambda e, smv=smv: e.reciprocal(out=smv[:, 12:16], in_=smv[:, 12:16]), T("sm", si), T("sm", si))
            for hh in range(4):
                pN, tN = pNs[hh]
                dve(lambda e, pN=pN, hh=hh, ci=ci, smv=smv: e.tensor_scalar(
                    out=hn[:, ci, hh * 256:(hh + 1) * 256], in0=pN[:, 0:256], scalar1=mv[:, hh, 0:1],
                    scalar2=smv[:, 12 + hh:13 + hh], op0=ALU.subtract, op1=ALU.mult),
                    [tN] + T("mv", hh) + T("sm", si), T("hn", ci))
                for i2 in range(2):
                    P.op("pe", lambda e, hh=hh, i2=i2, ci=ci, pT8b=pT8b: e.transpose(
                        out=pT8b[:, (2 * hh + i2) * 128:(2 * hh + i2 + 1) * 128],
                        in_=hn[:, ci, hh * 256 + i2 * 128:hh * 256 + (i2 + 1) * 128], identity=ident[:]),
                        reads=T("hn", ci) + T("ident"), writes=[tT8])
            dve(lambda e, pT8b=pT8b, cs=cs: e.tensor_tensor(
                out=ymT[:, :, cs], in0=pT8b[:, 0:1024].rearrange("p (k t) -> p k t", k=8), in1=bufB[:, :, cs], op=ALU.mult),
                [tT8] + [("bufB", k) for k in range(8)], [("ymT", k) for k in range(8)])

        def merge_branch(b, ysrc, ytok, first, last):
            Wb = WBR.ap()[l, b]
            for mp in range(4):
                stw, wtw = load_kcols(Wb, mp * 256, 256, cvtok(l, "br") + cvtok(l, "in"),
                                      extra=(C_G + b * 1024 + mp * 256, 256), extra_W=Win)
                wtg2 = wtw
                ww = kview(stw, 256)
                wgv = kview(stw, 256, off=2048)
                for mi in range(2):
                    m = mp * 2 + mi
                    pp, tp = nextbank()
                    pg, tg = nextbank()
                    for k in range(8):
                        mm(pg[:, :], wgv[:, k, mi * 128:(mi + 1) * 128], xn[:, k, :], k == 0, k == 7, [wtg2] + T("xn", k), tg)
                    for k in range(8):
                        mm(pp[:, :], ww[:, k, mi * 128:(mi + 1) * 128], ysrc[:, k, :], k == 0, k == 7,
                           [wtw] + [(ytok, k)], tp)
                    si = m % 2
                    act(lambda e, pg=pg, si=si: e.activation(out=sig[:, si, :], in_=pg[:, :], func=AF.Sigmoid),
                        [tg], T("sig", si))
                    if first:
                        dve(lambda e, pp=pp, si=si, m=m: e.tensor_tensor(out=fT[:, m, :], in0=pp[:, :], in1=sig[:, si, :],
                                                                         op=ALU.mult), [tp] + T("sig", si), T("fT", m))
                    else:
                        mi2 = m % 2
                        dve(lambda e, pp=pp, si=si, mi2=mi2: e.tensor_tensor(out=mtmp[:, mi2, :], in0=pp[:, :], in1=sig[:, si, :],
                                                                           op=ALU.mult), [tp] + T("sig", si), T("ctmp", mi2))
                        if not last:
                            P.op("dve", lambda e, m=m, mi2=mi2: e.tensor_tensor(out=fT[:, m, :], in0=fT[:, m, :],
                                                                                in1=mtmp[:, mi2, :], op=ALU.add),
                                 reads=T("fT", m) + T("ctmp", mi2), writes=T("fT", m))
                        else:
                            P.op("dve", lambda e, m=m, mi2=mi2: e.tensor_tensor(out=bufC[:, m, :], in0=fT[:, m, :],
                                                                                in1=mtmp[:, mi2, :], op=ALU.add),
                                 reads=T("fT", m) + T("ctmp", mi2), writes=T("bufC", m))


        lg = rowp[:, (l * 2) * D:(l * 2 + 1) * D]
        lb = rowp[:, (l * 2 + 1) * D:(l * 2 + 2) * D]
        gv4 = gv[:, :, :].rearrange("p a (b n) -> p (a b) n", b=2)
        for g in range(2):
            st, wt = load_kcols(Win, C_SV + g * 512, 512, cvtok(l, "in"))
            wv = kview(st, 512)
            si = g % 2
            smv = sm[:, si, :]
            for c in range(4):
                pk, tk = nextbank()
                for k in range(8):
                    mm(pk[:, :], xn[:, k, c * 128:(c + 1) * 128], wv[:, k, :], k == 0, k == 7, [wt] + T("xn", k), tk)
                act(lambda e, pk=pk, c=c: e.activation(out=gv4[:, c, :], in_=pk[:, :], func=AF.Gelu_apprx_tanh),
                    [tk], T("gv", c // 2))
                for g2 in range(2):
                    q = c * 2 + g2
                    dve(lambda e, c=c, g2=g2, q=q: e.bn_stats(out=st8[:, q, :], in_=gv4[:, c, g2 * 256:(g2 + 1) * 256]),
                        T("gv", c // 2), T("st8", q))
                    dve(lambda e, q=q: e.bn_aggr(out=mv8[:, q, :], in_=st8[:, q, :]), T("st8", q), T("mv8", q))
            act(lambda e, smv=smv: e.activation(out=smv[:, 0:8], in_=mv8[:, :, 1], func=AF.Sqrt, bias=EPS),
                [("mv8", q) for q in range(8)], T("sm", si))
            dve(lambda e, smv=smv: e.reciprocal(out=smv[:, 8:16], in_=smv[:, 0:8]), T("sm", si), T("sm", si))
            c0 = g * 512
            for c in range(4):
                for g2 in range(2):
                    q = c * 2 + g2
                    dve(lambda e, g2=g2, c=c, q=q, smv=smv, c0=c0: e.tensor_scalar(
                        out=vn[:, c, c0 + g2 * 256:c0 + (g2 + 1) * 256], in0=gv4[:, c, g2 * 256:(g2 + 1) * 256],
                        scalar1=mv8[:, q, 0:1], scalar2=smv[:, 8 + q:9 + q], op0=ALU.subtract, op1=ALU.mult),
                        T("gv", c // 2) + T("mv8", q) + T("sm", si), T("vn", c))
                dve(lambda e, c=c, c0=c0: e.tensor_tensor(out=vn[:, c, c0:c0 + 512], in0=vn[:, c, c0:c0 + 512],
                                                         in1=lg[:, c0:c0 + 512], op=ALU.mult),
                    T("vn", c) + T("rowp"), T("vn", c))
                dve(lambda e, c=c, c0=c0: e.tensor_tensor(out=vn[:, c, c0:c0 + 512], in0=vn[:, c, c0:c0 + 512],
                                                         in1=lb[:, c0:c0 + 512], op=ALU.add),
                    T("vn", c) + T("rowp"), T("vn", c))
        for c in range(4):
            cs = slice(c * 128, (c + 1) * 128)
            pa2 = [nextbank(), nextbank()]
            for j in range(8):
                g = j // 2
                pk, tk = pa2[j // 4]
                osl = slice((j % 4) * 128, (j % 4 + 1) * 128)
                wo = (l * 4 + g) * 128
                mm(pk[:, osl], vn[:, c, j * 128:(j + 1) * 128], wsT[:, wo:wo + 128], True, False, T("vn", c) + T("wsT"), tk)
                mm(pk[:, osl], onesb[0:1, :], bshi[0:1, wo:wo + 128], False, False, T("onesb") + T("bshi"), tk)
                mm(pk[:, osl], onesb[0:1, :], bslo[0:1, wo:wo + 128], False, True, T("onesb") + T("bslo"), tk)
            for hf in range(2):
                pk, tk = pa2[hf]
                dve(lambda e, pk=pk, hf=hf, cs=cs: e.tensor_tensor(
                    out=bufB[:, 4 * hf:4 * hf + 4, cs], in0=pk[:, :].rearrange("p (k t) -> p k t", k=4),
                    in1=bufC[:, 4 * hf:4 * hf + 4, cs], op=ALU.mult),
                    [tk] + [("bufC", k) for k in range(4 * hf, 4 * hf + 4)], [("bufB", k) for k in range(4 * hf, 4 * hf + 4)])
        for c in range(4):
            cs = slice(c * 128, (c + 1) * 128)
            pi = c % 2
            psc = [nextbank(), nextbank()]
            for hh in range(4):
                pk, tk = psc[hh // 2]
                osl = slice((hh % 2) * 256, (hh % 2 + 1) * 256)
                for i2 in range(2):
                    j = 2 * hh + i2
                    o0 = (l * 8 + j) * 256
                    mm(pk[:, osl], xqT[:, j, cs], kmT[:, o0:o0 + 256], i2 == 0, i2 == 1, T("xqT", j) + T("kmT", l), tk)
            si = c % 2
            smv = sm[:, si, :]
            for hf in range(2):
                pk, tk = psc[hf]
                dve(lambda e, pk=pk, hf=hf, smv=smv: e.tensor_reduce(
                    out=smv[:, 2 * hf:2 * hf + 2], in_=pk[:, :].rearrange("p (h m) -> p h m", h=2), axis=AX.X, op=ALU.max),
                    [tk], T("sm", si))
            dve(lambda e, smv=smv: e.tensor_scalar(out=smv[:, 4:8], in0=smv[:, 0:4], scalar1=-1.0 / 16.0, scalar2=None,
                                                   op0=ALU.mult), T("sm", si), T("sm", si))
            for hh in range(4):
                pk, tk = psc[hh // 2]
                osl = slice((hh % 2) * 256, (hh % 2 + 1) * 256)
                act(lambda e, pk=pk, osl=osl, hh=hh, pi=pi, smv=smv: e.activation(
                    out=pb[:, pi, hh * 256:(hh + 1) * 256], in_=pk[:, osl], func=AF.Exp, bias=smv[:, 4 + hh:5 + hh],
                    scale=1.0 / 16.0, accum_out=smv[:, 8 + hh:9 + hh]), [tk] + T("sm", si), T("hn", pi) + T("sm", si))
            dve(lambda e, smv=smv: e.reciprocal(out=smv[:, 12:16], in_=smv[:, 8:12]), T("sm", si), T("sm", si))
            dve(lambda e, pi=pi, smv=smv: e.tensor_tensor(
                out=pb[:, pi, :].rearrange("p (h m) -> p h m", h=4), in0=pb[:, pi, :].rearrange("p (h m) -> p h m", h=4),
                in1=smv[:, 12:16].unsqueeze(2).to_broadcast([128, 4, 256]), op=ALU.mult),
                T("hn", pi) + T("sm", si), T("hn", pi))
            pk8, tk8 = nextbank()
            pk8b = pk8[:, :].bitcast(BF16)
            for hh in range(4):
                for mc in range(2):
                    q8 = hh * 2 + mc
                    P.op("pe", lambda e, pk8b=pk8b, q8=q8, pi=pi, hh=hh, mc=mc: e.transpose(
                        out=pk8b[:, q8 * 128:(q8 + 1) * 128], in_=pb[:, pi, hh * 256 + mc * 128:hh * 256 + (mc + 1) * 128],
                        identity=ident[:]), reads=T("hn", pi) + T("ident"), writes=[tk8])
            act(lambda e, pk8b=pk8b, pi=pi: e.activation(out=pT[:, pi, :], in_=pk8b[:, 0:1024], func=AF.Copy),
                [tk8], T("pT", pi))
            pyx = [nextbank(), nextbank()]
            for j in range(8):
                hh = j // 2
                pk, tk = pyx[j // 4]
                osl = slice((j % 4) * 128, (j % 4 + 1) * 128)
                for mc in range(2):
                    o0 = (l * 2 + mc) * 1024 + j * 128
                    q8 = hh * 2 + mc
                    mm(pk[:, osl], vm[:, o0:o0 + 128], pT[:, pi, q8 * 128:(q8 + 1) * 128], mc == 0, mc == 1,
                       T("vm", l) + T("pT", pi), tk)
            for hf in range(2):
                pk, tk = pyx[hf]
                act(lambda e, pk=pk, hf=hf, cs=cs: e.activation(
                    out=bufA[:, 4 * hf:4 * hf + 4, cs], in_=pk[:, :].rearrange("p (k t) -> p k t", k=4), func=AF.Copy),
                    [tk], [("bufA", k) for k in range(4 * hf, 4 * hf + 4)])
        fence()
        merge_branch(0, ymT, "ymT", True, False)
        merge_branch(1, bufB, "bufB", False, False)
        merge_branch(2, bufA, "bufA", False, True)
        Wo = WOUT.ap()[l]
        for g in range(2):
            st, wt = load_kcols(Wo, g * 512, 512, cvtok(l, "out"))
            wv = kview(st, 512)
            for jj in range(4):
                m = g * 4 + jj
                pk, tk = nextbank()
                for k in range(8):
                    mm(pk[:, :], wv[:, k, jj * 128:(jj + 1) * 128], bufC[:, k, :], k == 0, k == 7, [wt] + T("bufC", k), tk)
                act(lambda e, pk=pk, m=m: e.activation(out=fT[:, m, :], in_=pk[:, :], func=AF.Copy), [tk], T("fT", m))
        postnorm_residual(l, 3, False)

    setup_kv()
    for t in range(NT):
        ts = slice(t * TM, (t + 1) * TM)
        for k in range(8):
            P.dma("sp", h[:, k, :], xT_d.ap()[k * 128:(k + 1) * 128, ts], writes=T("h", k), stream="ld_h%d" % k)
        for l in range(L):
            ffn(l, 0)
            if cfg.stages == "ffn1":
                continue
            fence()
            mixer(l)
            fence()
            if cfg.stages == "mix":
                continue
            ffn(l, 1)
        for k in range(8):
            P.dma("sp", yT_d.ap()[k * 128:(k + 1) * 128, ts], h[:, k, :], reads=T("h", k), writes=[("yT", k)], stream="st_y%d" % k)
    pump(len(jobs))
    P.barrier("sp", [("yT", k) for k in range(8)])
    P.emit()
    return nc, P


def prep_shared(inp, cfg):
    L = cfg.L
    f = lambda a: np.ascontiguousarray(np.asarray(a, dtype=np.float32))
    ng = f(inp["norm_g"])[:L]
    cw = f(inp["conv_w"])[:L]
    cb = f(inp["conv_b"])[:L]
    mg = f(inp["mlstm_norm_g"])[:L]
    pv = np.concatenate([
        ng.reshape(L, 7, 8, 128).transpose(3, 0, 1, 2).reshape(128, L * 56),
        cw.reshape(L, 4, 8, 128).transpose(3, 0, 1, 2).reshape(128, L * 32),
        cb.reshape(L, 8, 128).transpose(2, 0, 1).reshape(128, L * 8),
        mg.reshape(L, 8, 128).transpose(2, 0, 1).reshape(128, L * 8)], axis=1)
    lg = f(inp["sgu_ln_g"])[:L]
    lb = f(inp["sgu_ln_b"])[:L]
    rowp = np.stack([lg, lb], axis=1).reshape(1, L * 2 * D)
    rowp = np.ascontiguousarray(np.broadcast_to(rowp, (128, L * 2 * D)))
    ws = f(inp["sgu_w_s"])[:L]
    wsT = np.ascontiguousarray(ws.transpose(3, 0, 1, 2).reshape(128, L * 4 * 128))
    bs = f(inp["sgu_b_s"])[:L].reshape(1, L * 512)
    bif = np.stack([f(inp["b_i"])[:L], f(inp["b_f"])[:L]], axis=1)
    bif = np.ascontiguousarray(bif.transpose(2, 0, 1).reshape(4, 2 * L))
    ident = np.eye(128, dtype=np.float32).astype(ml_dtypes.bfloat16)
    maskle = np.triu(np.ones((128, 128), dtype=np.float32)).astype(ml_dtypes.bfloat16)
    sel = np.zeros((4, 4, 128), dtype=np.float32)
    for hh in range(4):
        sel[hh, hh, :] = 1.0
    sel = sel.reshape(4, 512)
    return {
        "w_gu": f(inp["w_ffn_gu"])[:L], "w_dn": f(inp["w_ffn_down"])[:L], "w_in": f(inp["w_in"])[:L],
        "w_kv": f(inp["w_mkv"])[:L], "w_br": f(inp["w_branch"])[:L], "w_out": f(inp["w_out"])[:L],
        "pvec": np.ascontiguousarray(pv), "rowp": rowp, "wsT": wsT, "bs": np.ascontiguousarray(bs), "bif": bif,
        "ident": ident, "maskle": maskle, "sel": sel,
    }


_CACHE = {}


def run(inp, cfg, n_cores, trace=False):
    key = (cfg.S, cfg.L, cfg.stages, cfg.preconvert)
    if key not in _CACHE:
        _CACHE[key] = build_program(cfg)
    nc, P = _CACHE[key]
    shared = prep_shared(inp, cfg)
    x = np.asarray(inp["x"], dtype=np.float32)
    mem = np.asarray(inp["mem"], dtype=np.float32)
    in_maps = []
    for b in range(n_cores):
        m = dict(shared)
        m["xT"] = np.ascontiguousarray(x[b, :cfg.S].T)
        m["memT"] = np.ascontiguousarray(mem[b].T)
        in_maps.append(m)
    res = run_bass_kernel_spmd(nc, in_maps, core_ids=list(range(n_cores)), trace=trace)
    out = np.stack([np.ascontiguousarray(r["yT"].T) for r in res.results], axis=0)
    return out.astype(np.float32), res


def kernel(x, mem, norm_g, w_ffn_gu, w_ffn_down, w_in, conv_w, conv_b, b_i, b_f, mlstm_norm_g, sgu_ln_g, sgu_ln_b,
           sgu_w_s, sgu_b_s, w_mkv, w_branch, w_out):
    inp = dict(x=x, mem=mem, norm_g=norm_g, w_ffn_gu=w_ffn_gu, w_ffn_down=w_ffn_down, w_in=w_in, conv_w=conv_w,
               conv_b=conv_b, b_i=b_i, b_f=b_f, mlstm_norm_g=mlstm_norm_g, sgu_ln_g=sgu_ln_g, sgu_ln_b=sgu_ln_b,
               sgu_w_s=sgu_w_s, sgu_b_s=sgu_b_s, w_mkv=w_mkv, w_branch=w_branch, w_out=w_out)
    cfg = Cfg(S=4096, L=2, stages="all", preconvert=True)
    out, _ = run(inp, cfg, 8)
    return out
```

```python
import contextlib
import numpy as np
import ml_dtypes
import concourse.bass as bass
import concourse.mybir as mybir
from concourse.bass_utils import run_bass_kernel_spmd

F32 = mybir.dt.float32
BF16 = mybir.dt.bfloat16
AF = mybir.ActivationFunctionType
ALU = mybir.AluOpType
AX = mybir.AxisListType

D = 1024
DFF = 2816
MEM = 256
WIN = 9224
TM = 512
C_QK, C_V, C_O, C_I, C_F, C_U, C_SV, C_XQ, C_G = 0, 1024, 2048, 3072, 3076, 3080, 4104, 5128, 6152
EPS = 1e-6
NSLOT = 3
SLOTW = 4096

ENGINES = ("pe", "act", "dve", "pool", "sp")


class Op:
    __slots__ = ("eng", "fn", "deps", "is_dma", "stream", "signaled", "sem", "val", "clock")

    def __init__(self, eng, fn, is_dma=False, stream=None):
        self.eng = eng
        self.fn = fn
        self.deps = []
        self.is_dma = is_dma
        self.stream = stream
        self.signaled = False
        self.sem = None
        self.val = 0
        self.clock = None


class Prog:
    def __init__(self, nc):
        self.nc = nc
        self.ops = []
        self.by_eng = {e: [] for e in ENGINES}
        self.last_writer = {}
        self.readers = {}

    def _add_dep(self, op, d):
        if d is None or d is op:
            return
        if d.is_dma and op.is_dma and d.stream == op.stream:
            return
        if d not in op.deps:
            op.deps.append(d)

    def op(self, eng, fn, reads=(), writes=(), dma=False, stream=None):
        o = Op(eng, fn, is_dma=dma, stream=stream)
        for t in reads:
            w = self.last_writer.get(t)
            if w is not None and w.eng == eng and eng == "pe" and not w.is_dma and not dma:
                continue
            self._add_dep(o, w)
        for t in writes:
            w = self.last_writer.get(t)
            if w is not None and not (w.eng == eng and not w.is_dma and not dma):
                self._add_dep(o, w)
            for r in self.readers.get(t, ()):
                if not (r.eng == eng and not r.is_dma and not dma):
                    self._add_dep(o, r)
        for t in reads:
            rl = self.readers.setdefault(t, [])
            if not dma:
                for i2 in range(len(rl)):
                    if rl[i2].eng == eng and not rl[i2].is_dma:
                        rl[i2] = o
                        break
                else:
                    rl.append(o)
            else:
                rl.append(o)
        for t in writes:
            self.last_writer[t] = o
            self.readers[t] = []
        self.ops.append(o)
        self.by_eng[eng].append(o)
        return o

    def dma(self, q, out, in_, reads=(), writes=(), stream=None):
        return self.op(q, lambda e: e.dma_start(out=out, in_=in_), reads=reads, writes=writes,
                       dma=True, stream=stream)

    def barrier(self, eng, toks):
        return self.op(eng, None, reads=toks)

    def emit(self):
        nc = self.nc
        for o in self.ops:
            for d in o.deps:
                d.signaled = True
        stack = contextlib.ExitStack()
        eng_sem = {e: stack.enter_context(nc.semaphore("s_" + e)) for e in ENGINES}
        stream_sem = {}
        for o in self.ops:
            if o.is_dma and o.stream not in stream_sem:
                stream_sem[o.stream] = stack.enter_context(nc.semaphore("d_" + o.stream))
        cnt = {e: 0 for e in ENGINES}
        scnt = {s: 0 for s in stream_sem}
        for o in self.ops:
            if o.is_dma:
                scnt[o.stream] += 1
                o.sem = stream_sem[o.stream]
                o.val = 16 * scnt[o.stream]
            elif o.signaled and o.fn is not None:
                cnt[o.eng] += 1
                o.sem = eng_sem[o.eng]
                o.val = cnt[o.eng]
        eng_clock = {e: {} for e in ENGINES}
        waits = {}
        nw = 0
        for o in self.ops:
            ck = eng_clock[o.eng]
            w = []
            for d in o.deps:
                key = id(d.sem)
                if ck.get(key, 0) >= d.val:
                    continue
                w.append((d.sem, d.val))
                for k2, v2 in d.clock.items():
                    if ck.get(k2, 0) < v2:
                        ck[k2] = v2
                ck[key] = d.val
            waits[id(o)] = w
            nw += len(w)
            o.clock = dict(ck) if o.signaled or o.is_dma else None
        self.n_waits = nw
        self.sem_counts = dict(cnt)
        handles = {"pe": "tensor", "act": "scalar", "dve": "vector", "pool": "gpsimd", "sp": "sync"}
        with nc.Block() as block:
            for e in ENGINES:
                ops = self.by_eng[e]
                if not ops:
                    continue

                def body(eh, ops=ops):
                    for o in ops:
                        for (s, v) in waits[id(o)]:
                            eh.wait_ge(s, v)
                        if o.fn is None:
                            continue
                        ins = o.fn(eh)
                        if o.is_dma:
                            ins.then_inc(o.sem, 16)
                        elif o.signaled:
                            ins.then_inc(o.sem, 1)
                getattr(block, handles[e])(body)
        stack.close()


class Cfg:
    def __init__(self, S=4096, L=2, stages="all", preconvert=True):
        self.S = S
        self.L = L
        self.stages = stages
        self.preconvert = preconvert


def build_program(cfg):
    S, L = cfg.S, cfg.L
    NT = S // TM
    nc = bass.Bass("TRN2", target_bir_lowering=False)
    P = Prog(nc)

    def dram_in(name, shape, dt=F32):
        return nc.dram_tensor(name, list(shape), dt, kind="ExternalInput")

    xT_d = dram_in("xT", [D, S])
    memT_d = dram_in("memT", [D, MEM])
    wgu_d = dram_in("w_gu", [L, 2, D, 2 * DFF])
    wdn_d = dram_in("w_dn", [L, 2, DFF, D])
    win_d = dram_in("w_in", [L, D, WIN])
    wkv_d = dram_in("w_kv", [L, D, 2 * D])
    wbr_d = dram_in("w_br", [L, 3, D, D])
    wout_d = dram_in("w_out", [L, D, D])
    NPV = 104 * L
    pv_d = dram_in("pvec", [128, NPV])
    rowp_d = dram_in("rowp", [128, L * 2 * D])
    wsT_d = dram_in("wsT", [128, L * 4 * 128])
    bs_d = dram_in("bs", [1, L * 512])
    bif_d = dram_in("bif", [4, 2 * L])
    ident_d = dram_in("ident", [128, 128], BF16)
    mask_d = dram_in("maskle", [128, 128], BF16)
    sel_d = dram_in("sel", [4, 512])
    yT_d = nc.dram_tensor("yT", [D, S], F32, kind="ExternalOutput")

    if cfg.preconvert:
        sc = lambda name, shape: nc.dram_tensor(name, list(shape), BF16, kind="Internal")
        wgu_b = sc("wgu_b", [L, 2, D, 2 * DFF])
        wdn_b = sc("wdn_b", [L, 2, DFF, D])
        win_b = sc("win_b", [L, D, WIN])
        wkv_b = sc("wkv_b", [L, D, 2 * D])
        wbr_b = sc("wbr_b", [L, 3, D, D])
        wout_b = sc("wout_b", [L, D, D])

    def sb(name, shape, dt):
        return nc.alloc_sbuf_tensor("sb_" + name, list(shape), dt)

    ident = sb("ident", [128, 128], BF16)
    onesb = sb("onesb", [128, 128], BF16)
    maskb = sb("maskb", [128, 128], BF16)
    sel = sb("sel", [4, 512], F32)
    pv = sb("pv", [128, NPV], F32)
    pvh = sb("pvh", [128, NPV], F32)
    rowp = sb("rowp", [128, L * 2 * D], BF16)
    wsT = sb("wsT", [128, L * 4 * 128], BF16)
    bshi = sb("bshi", [1, L * 512], BF16)
    bslo = sb("bslo", [1, L * 512], BF16)
    bif = sb("bif", [4, 2 * L], F32)
    ones4 = sb("ones4", [4, 512], F32)
    Cst = sb("Cst", [128, L * 4 * 257], F32)
    tails = sb("tails", [128, L * 8 * 3], F32)
    Bprev = sb("Bprev", [4, L], F32)
    MU = sb("MU", [4, L * 5], F32)
    kmT = sb("kmT", [128, L * 8 * 256], BF16)
    vm = sb("vm", [128, L * 2 * 1024], BF16)
    h = sb("h", [128, 8, TM], F32)
    xn = sb("xn", [128, 8, TM], BF16)
    sq = sb("sq", [128, 4, TM], BF16)
    rstd = sb("rstd", [128, TM], F32)
    wslot = [sb("wslot%d" % i, [128, SLOTW], BF16) for i in range(NSLOT)]
    a012 = sb("a012", [128, 12288], BF16)
    bufA = a012[:, 0:4096].rearrange("p (k t) -> p k t", k=8)
    bufB = a012[:, 4096:8192].rearrange("p (k t) -> p k t", k=8)
    bufC = a012[:, 8192:12288].rearrange("p (k t) -> p k t", k=8)
    hid = a012[:, 0:22 * TM].rearrange("p (k t) -> p k t", k=22)
    ymT_t = sb("ymT", [128, 8 * TM], BF16)
    ymT = ymT_t[:, :].rearrange("p (k t) -> p k t", k=8)
    sil = ymT_t[:, 0:4 * TM].bitcast(F32).rearrange("p (k t) -> p k t", k=2)
    xqT = sb("xqT", [128, 8, TM], BF16)
    a5 = sb("a5", [128, 10320], BF16)
    vn = a5[:, 0:4096].rearrange("p (c n) -> p c n", c=4)
    kp = a5[:, 4096:6144].rearrange("p (c n) -> p c n", c=4)
    vaug = a5[:, 6144:6144 + 4112].rearrange("p (c h n) -> p c h n", c=4, h=4)
    fT = a5[:, 0:8192].bitcast(F32).rearrange("p (k t) -> p k t", k=8)
    gv = sb("gv", [128, 2, 1024], F32)
    zb = sb("zb", [128, 2, TM + 3], F32)
    ctmp = sb("ctmp", [128, 2, TM], F32)
    mtmp = ctmp
    sig = sb("sig", [128, 2, TM], BF16)
    Cbf = sb("Cbf", [128, 4, 4 * 257], BF16)
    ST = sb("ST", [128, 4, 512], BF16)
    hn = sb("hn", [128, 2, 1024], BF16)
    pb = hn
    pT = sb("pT", [128, 2, 1024], BF16)
    gI = sb("gI", [4, TM], F32)
    gF = gv[0:4, 0, 0:TM]
    gT1 = gv[0:4, 0, TM:2 * TM]
    gT2 = gv[0:4, 1, 0:TM]
    gB = gv[0:4, 1, TM:2 * TM]
    gW = sb("gW", [4, TM], F32)
    gE = sb("gE", [4, TM], F32)
    mx4 = sb("mx4", [4, 4], F32)
    dec4 = sb("dec4", [4, 4], F32)
    decb = sb("decb", [128, 16], F32)
    tokW = sb("tokW", [128, 4, 8], F32)
    st6 = sb("st6", [128, 4, 6], F32)
    st8 = sb("st8", [128, 8, 6], F32)
    mv8 = sb("mv8", [128, 8, 2], F32)
    mv = sb("mv", [128, 4, 2], F32)
    sm = sb("sm", [128, 2, 16], F32)
    dummy = sb("dummy", [1, 8], F32)
    epsb = sb("epsb", [128, 1], F32)

    banks = [nc.alloc_psum_tensor("bank%d" % i, [128, 512], F32) for i in range(8)]
    bank_rr = [0]

    def nextbank(lo=0, hi=6):
        i = lo + bank_rr[0] % (hi - lo)
        bank_rr[0] += 1
        return banks[i], ("bank", i)

    def T(name, k=None, c=None):
        if k is None:
            return [(name,)]
        if c is None:
            return [(name, k)]
        return [(name, k, c)]

    ARENA_TOKS = []

    def reg_arena(toks):
        ARENA_TOKS.extend(toks)
        return toks

    for k in range(8):
        reg_arena([("bufA", k), ("bufB", k), ("bufC", k), ("ymT", k), ("fT", k)])
    for j in range(22):
        reg_arena([("hid", j)])
    reg_arena([("sil", 0), ("sil", 1)])
    for c in range(4):
        reg_arena([("vn", c), ("kp", c), ("vaug", c)])

    def fence():
        P.op("pool", lambda e: e.memset(dummy[:, 0:1], 0.0), writes=list(ARENA_TOKS))

    def pvcol(kind, l, a=0, b=0):
        if kind == "ng":
            return (l * 7 + a) * 8 + b
        if kind == "cw":
            return L * 56 + (l * 4 + a) * 8 + b
        if kind == "cb":
            return L * 88 + l * 8 + a
        if kind == "mg":
            return L * 96 + l * 8 + a
        raise ValueError

    P.dma("sp", ident[:], ident_d.ap(), writes=T("ident"), stream="c0")
    P.dma("sp", maskb[:], mask_d.ap(), writes=T("maskb"), stream="c2")
    P.dma("sp", sel[:], sel_d.ap(), writes=T("sel"), stream="c3")
    P.dma("sp", pv[:], pv_d.ap(), writes=T("pv"), stream="c4")
    P.dma("sp", bif[:], bif_d.ap(), writes=T("bif"), stream="c5")
    P.dma("pool", rowp[:], rowp_d.ap(), writes=T("rowp"), stream="c1")
    gvflat = gv[:, :, :].rearrange("p a b -> p (a b)")
    bsf = gv[0:1, 1, 0:L * 512]
    P.dma("sp", bsf, bs_d.ap(), writes=T("bsf") + T("gv", 1), stream="c6")
    P.dma("sp", gvflat[:, 0:L * 512], wsT_d.ap(), writes=T("gv", 0), stream="c7")
    for l in range(L):
        for g in range(4):
            o0 = (l * 4 + g) * 128
            P.op("dve", lambda e, o0=o0: e.tensor_tensor(out=wsT[:, o0:o0 + 128], in0=gvflat[:, o0:o0 + 128],
                                                         in1=maskb[:], op=ALU.mult),
                 reads=T("gv", 0) + T("maskb"), writes=T("wsT"))
    P.op("dve", lambda e: e.memset(onesb[:], 1.0), writes=T("onesb"))
    P.op("dve", lambda e: e.memset(epsb[:], EPS), writes=T("epsb"))
    P.op("dve", lambda e: e.memset(ones4[:], 1.0), writes=T("ones4"))
    P.op("dve", lambda e: e.memset(Cst[:], 0.0), writes=[("C", l) for l in range(L)])
    P.op("dve", lambda e: e.memset(tails[:], 0.0), writes=[("tails", l) for l in range(L)])
    P.op("dve", lambda e: e.memset(Bprev[:], 0.0), writes=[("Bprev", l) for l in range(L)])
    P.op("dve", lambda e: e.memset(MU[:], -1e30), writes=[("MU", l) for l in range(L)])
    P.op("dve", lambda e: e.tensor_scalar(out=pvh[:], in0=pv[:], scalar1=0.5, scalar2=None, op0=ALU.mult),
         reads=T("pv"), writes=T("pvh"))
    P.op("dve", lambda e: e.tensor_copy(out=bshi[:], in_=bsf), reads=T("bsf"), writes=T("bshi"))
    P.op("dve", lambda e: e.tensor_tensor(out=bslo[:], in0=bsf, in1=bshi[:], op=ALU.subtract),
         reads=T("bsf") + T("bshi"), writes=T("bslo") + T("gv", 1))

    jobs = []
    job_last = {}
    jp = [0]

    def cvt(dst, src, tok, stream):
        job_last[tok] = len(jobs)
        jobs.append((dst, src, tok, stream))

    def pump(n):
        while n > 0 and jp[0] < len(jobs):
            dst, src, tok, stream = jobs[jp[0]]
            P.dma("pool", dst, src, writes=[tok], stream=stream)
            jp[0] += 1
            n -= 1

    def ensure(tok):
        if tok in job_last:
            while jp[0] <= job_last[tok]:
                pump(1)

    if cfg.preconvert:
        for l in range(L):
            for k in range(8):
                cvt(wkv_b.ap()[l, k * 128:(k + 1) * 128, :], wkv_d.ap()[l, k * 128:(k + 1) * 128, :],
                    ("cv", l, "kv"), "cv%dkv" % l)
        for l in range(L):
            for f in range(2):
                if f == 1:
                    for k in range(8):
                        for hf in range(4):
                            c0 = hf * 2306
                            cvt(win_b.ap()[l, k * 128:(k + 1) * 128, c0:c0 + 2306],
                                win_d.ap()[l, k * 128:(k + 1) * 128, c0:c0 + 2306], ("cv", l, "in"), "cv%din" % l)
                    for b in range(3):
                        for k in range(8):
                            cvt(wbr_b.ap()[l, b, k * 128:(k + 1) * 128, :], wbr_d.ap()[l, b, k * 128:(k + 1) * 128, :],
                                ("cv", l, "br"), "cv%dbr" % l)
                    for k in range(8):
                        cvt(wout_b.ap()[l, k * 128:(k + 1) * 128, :], wout_d.ap()[l, k * 128:(k + 1) * 128, :],
                            ("cv", l, "out"), "cv%dout" % l)
                for k in range(8):
                    for hf in range(2):
                        cvt(wgu_b.ap()[l, f, k * 128:(k + 1) * 128, hf * DFF:(hf + 1) * DFF],
                            wgu_d.ap()[l, f, k * 128:(k + 1) * 128, hf * DFF:(hf + 1) * DFF],
                            ("cv", l, "gu", f), "cv%dgu%d" % (l, f))
                for j in range(22):
                    cvt(wdn_b.ap()[l, f, j * 128:(j + 1) * 128, :], wdn_d.ap()[l, f, j * 128:(j + 1) * 128, :],
                        ("cv", l, "dn", f), "cv%ddn%d" % (l, f))
        WGU, WDN, WINB, WKV, WBR, WOUT = wgu_b, wdn_b, win_b, wkv_b, wbr_b, wout_b
        wq = "sp"
    else:
        WGU, WDN, WINB, WKV, WBR, WOUT = wgu_d, wdn_d, win_d, wkv_d, wbr_d, wout_d
        wq = "pool"

    def cvtok(l, kind, f=None):
        if not cfg.preconvert:
            return []
        return [("cv", l, kind)] if f is None else [("cv", l, kind, f)]

    slot_rr = [0]

    def load_slot(parts, rtoks):
        i = slot_rr[0] % NSLOT
        slot_rr[0] += 1
        st = wslot[i]
        for tk in rtoks:
            ensure(tk)
        pump(3)
        for (dstf, src) in parts:
            P.dma(wq, dstf(st), src, reads=rtoks, writes=[("w", i)], stream="w%d" % i)
        return st, ("w", i)

    def kview(st, ncols, off=0):
        return st[:, off:off + 8 * ncols].rearrange("p (k c) -> p k c", k=8)

    def load_kcols(W_ap2d, c0, ncols, rtoks, extra=None, extra_W=None):
        parts = [(lambda st: kview(st, ncols), W_ap2d[:, c0:c0 + ncols].rearrange("(k p) c -> p k c", p=128))]
        if extra is not None:
            c1, n1 = extra
            W2 = W_ap2d if extra_W is None else extra_W
            parts.append((lambda st: kview(st, n1, off=8 * ncols),
                          W2[:, c1:c1 + n1].rearrange("(k p) c -> p k c", p=128)))
        return load_slot(parts, rtoks)

    def mm(out, lhsT, rhs, start, stop, reads, wtok):
        P.op("pe", lambda e: e.matmul(out, lhsT=lhsT, rhs=rhs, start=start, stop=stop), reads=reads, writes=[wtok])

    def rms_stats(src_fn, src_toks_fn, ncols=TM, scale=1.0 / D):
        bk, btok = banks[7], ("bank", 7)
        for k in range(8):
            if k % 2 == 0:
                P.op("act", lambda e, k=k: e.activation(out=sq[:, k % 4, 0:ncols], in_=src_fn(k), func=AF.Square),
                     reads=src_toks_fn(k), writes=T("sq", k % 4))
            else:
                P.op("dve", lambda e, k=k: e.tensor_tensor(out=sq[:, k % 4, 0:ncols], in0=src_fn(k), in1=src_fn(k),
                                                           op=ALU.mult),
                     reads=src_toks_fn(k), writes=T("sq", k % 4))
            mm(bk[:, 0:ncols], onesb[:], sq[:, k % 4, 0:ncols], k == 0, k == 7, T("sq", k % 4) + T("onesb"), btok)
        P.op("act", lambda e: e.activation(out=rstd[:, 0:ncols], in_=bk[:, 0:ncols], func=AF.Ln, bias=epsb[:, 0:1], scale=scale),
             reads=[btok] + T("epsb"), writes=T("rstd"))
        P.op("act", lambda e: e.activation(out=rstd[:, 0:ncols], in_=rstd[:, 0:ncols], func=AF.Exp, scale=-0.5),
             reads=T("rstd"), writes=T("rstd"))

    def prenorm(l, n):
        rms_stats(lambda k: h[:, k, :], lambda k: T("h", k))
        for k in range(8):
            col = pvcol("ng", l, n, k)
            P.op("dve", lambda e, k=k, col=col: e.scalar_tensor_tensor(
                out=xn[:, k, :], in0=h[:, k, :], scalar=pv[:, col:col + 1], in1=rstd[:, :], op0=ALU.mult, op1=ALU.mult),
                reads=T("h", k) + T("rstd") + T("pv"), writes=T("xn", k))

    def postnorm_residual(l, n, half):
        rms_stats(lambda k: fT[:, k, :], lambda k: T("fT", k))
        pvt = pvh if half else pv
        ptok = T("pvh") if half else T("pv")
        for k in range(8):
            col = pvcol("ng", l, n, k)
            P.op("dve", lambda e, k=k, col=col: e.scalar_tensor_tensor(
                out=fT[:, k, :], in0=fT[:, k, :], scalar=pvt[:, col:col + 1], in1=rstd[:, :], op0=ALU.mult, op1=ALU.mult),
                reads=T("fT", k) + T("rstd") + ptok, writes=T("fT", k))
            P.op("dve", lambda e, k=k: e.tensor_tensor(out=h[:, k, :], in0=h[:, k, :], in1=fT[:, k, :], op=ALU.add),
                 reads=T("fT", k) + T("h", k), writes=T("h", k))

    def ffn(l, f):
        prenorm(l, 0 if f == 0 else 4)
        Wgu = WGU.ap()[l, f]
        Wdn = WDN.ap()[l, f]
        for grp in range(11):
            st, wt = load_kcols(Wgu, grp * 256, 256, cvtok(l, "gu", f), extra=(DFF + grp * 256, 256))
            wa = kview(st, 256)
            wb = kview(st, 256, off=2048)
            for jj in range(2):
                j = grp * 2 + jj
                pa, ta = nextbank()
                pbk, tb = nextbank()
                for k in range(8):
                    mm(pa[:, :], wa[:, k, jj * 128:(jj + 1) * 128], xn[:, k, :], k == 0, k == 7, [wt] + T("xn", k), ta)
                for k in range(8):
                    mm(pbk[:, :], wb[:, k, jj * 128:(jj + 1) * 128], xn[:, k, :], k == 0, k == 7, [wt] + T("xn", k), tb)
                P.op("act", lambda e, pa=pa, j=j: e.activation(out=sil[:, j % 2, :], in_=pa[:, :], func=AF.Silu),
                     reads=[ta], writes=T("sil", j % 2))
                P.op("dve", lambda e, pbk=pbk, j=j: e.tensor_tensor(out=hid[:, j, :], in0=sil[:, j % 2, :], in1=pbk[:, :],
                                                                  op=ALU.mult),
                     reads=[tb] + T("sil", j % 2), writes=T("hid", j))
        for mp in range(4):
            pm = [nextbank(), nextbank()]
            for half in range(2):
                parts = [(lambda st: st[:, 0:11 * 256].rearrange("p (j c) -> p j c", j=11),
                          Wdn[half * 1408:(half + 1) * 1408, mp * 256:(mp + 1) * 256].rearrange("(j p) c -> p j c", p=128))]
                st, wt = load_slot(parts, cvtok(l, "dn", f))
                wv = st[:, 0:11 * 256].rearrange("p (j c) -> p j c", j=11)
                for mi in range(2):
                    pk, tk = pm[mi]
                    for jj in range(11):
                        j = half * 11 + jj
                        mm(pk[:, :], wv[:, jj, mi * 128:(mi + 1) * 128], hid[:, j, :], j == 0, j == 21,
                           [wt] + T("hid", j), tk)
            for mi in range(2):
                m = mp * 2 + mi
                pk, tk = pm[mi]
                P.op("act", lambda e, pk=pk, m=m: e.activation(out=fT[:, m, :], in_=pk[:, :], func=AF.Copy),
                     reads=[tk], writes=T("fT", m))
        postnorm_residual(l, 1 if f == 0 else 5, True)

    def setup_kv():
        mt = h
        for k in range(8):
            P.dma("sp", h[:, k, 0:MEM], memT_d.ap()[k * 128:(k + 1) * 128, :], writes=T("h", k), stream="ld_h%d" % k)
        for l in range(L):
            rms_stats(lambda k: h[:, k, 0:MEM], lambda k: T("h", k), ncols=MEM)
            for k in range(8):
                col = pvcol("ng", l, 6, k)
                P.op("dve", lambda e, k=k, col=col: e.scalar_tensor_tensor(
                    out=xn[:, k, 0:MEM], in0=h[:, k, 0:MEM], scalar=pv[:, col:col + 1], in1=rstd[:, 0:MEM],
                    op0=ALU.mult, op1=ALU.mult), reads=T("h", k) + T("rstd") + T("pv"), writes=T("xn", k))
            Wkv = WKV.ap()[l]
            for g in range(4):
                st, wt = load_kcols(Wkv, g * 512, 512, cvtok(l, "kv"))
                wv = kview(st, 512)
                if g < 2:
                    for jj in range(4):
                        j = g * 4 + jj
                        pk, tk = nextbank()
                        for k in range(8):
                            mm(pk[:, 0:MEM], wv[:, k, jj * 128:(jj + 1) * 128], xn[:, k, 0:MEM], k == 0, k == 7,
                               [wt] + T("xn", k), tk)
                        o0 = (l * 8 + j) * 256
                        P.op("act", lambda e, pk=pk, o0=o0: e.activation(out=kmT[:, o0:o0 + 256], in_=pk[:, 0:MEM], func=AF.Copy),
                             reads=[tk], writes=T("kmT", l))
                else:
                    for mc in range(2):
                        pk, tk = nextbank()
                        for k in range(8):
                            mm(pk[:, :], xn[:, k, mc * 128:(mc + 1) * 128], wv[:, k, :], k == 0, k == 7,
                               [wt] + T("xn", k), tk)
                        o0 = (l * 2 + mc) * 1024 + (g - 2) * 512
                        P.op("act", lambda e, pk=pk, o0=o0: e.activation(out=vm[:, o0:o0 + 512], in_=pk[:, :], func=AF.Copy),
                             reads=[tk], writes=T("vm", l))

    def mixer(l):
        prenorm(l, 2)
        Win = WINB.ap()[l]
        Ct = Cst[:, l * 1028:(l + 1) * 1028].rearrange("p (h n) -> p h n", h=4)
        stg, wtg = load_kcols(Win, C_I, 8, cvtok(l, "in"))
        wg8 = kview(stg, 8)
        bk6, t6 = banks[6], ("bank", 6)
        for k in range(8):
            mm(bk6[0:4, 0:TM], wg8[:, k, 0:4], xn[:, k, :], k == 0, k == 7, [wtg] + T("xn", k), t6)
        bk7, t7 = banks[7], ("bank", 7)
        for k in range(8):
            mm(bk7[0:4, 0:TM], wg8[:, k, 4:8], xn[:, k, :], k == 0, k == 7, [wtg] + T("xn", k), t7)
        bi = bif[:, 2 * l:2 * l + 1]
        bfp = bif[:, 2 * l + 1:2 * l + 2]
        dve = lambda fn, r, w: P.op("dve", fn, reads=r, writes=w)
        act = lambda fn, r, w: P.op("act", fn, reads=r, writes=w)
        dve(lambda e: e.tensor_scalar(out=gF, in0=bk7[0:4, 0:TM], scalar1=bfp, scalar2=None, op0=ALU.add),
            [t7] + T("bif"), T("gv", 0))
        act(lambda e: e.activation(out=gT1, in_=gF, func=AF.Abs), T("gv", 0), T("gv", 0))
        act(lambda e: e.activation(out=gT2, in_=gT1, func=AF.Exp, scale=-1.0), T("gv", 0), T("gv", 1))
        act(lambda e: e.activation(out=gT2, in_=gT2, func=AF.Ln, bias=1.0), T("gv", 1), T("gv", 1))
        dve(lambda e: e.tensor_single_scalar(out=gT1, in_=gF, scalar=0.0, op=ALU.min), T("gv", 0), T("gv", 0))
        dve(lambda e: e.tensor_tensor(out=gF, in0=gT1, in1=gT2, op=ALU.subtract), T("gv", 0) + T("gv", 1), T("gv", 0))
        dve(lambda e: e.tensor_tensor_scan(out=gB, data0=ones4[:], data1=gF, initial=Bprev[:, l:l + 1],
                                           op0=ALU.mult, op1=ALU.add), T("gv", 0) + T("ones4") + [("Bprev", l)], T("gv", 1))
        dve(lambda e: e.tensor_copy(out=Bprev[:, l:l + 1], in_=gB[:, TM - 1:TM]), T("gv", 1), [("Bprev", l)])
        dve(lambda e: e.scalar_tensor_tensor(out=gI[:], in0=bk6[0:4, 0:TM], scalar=bi, in1=gB, op0=ALU.add,
                                             op1=ALU.subtract), [t6] + T("bif") + T("gv", 1), T("gI"))
        dve(lambda e: e.tensor_reduce(out=mx4[:], in_=gI[:, :].rearrange("p (c t) -> p c t", c=4), axis=AX.X, op=ALU.max),
            T("gI"), T("mx4"))
        mul = MU[:, l * 5:(l + 1) * 5]
        dve(lambda e: e.tensor_tensor_scan(out=mul[:, 1:5], data0=ones4[:, 0:4], data1=mx4[:], initial=mul[:, 0:1],
                                           op0=ALU.mult, op1=ALU.max), T("mx4") + T("ones4") + [("MU", l)], [("MU", l)])
        dve(lambda e: e.tensor_tensor(out=dec4[:], in0=mul[:, 0:4], in1=mul[:, 1:5], op=ALU.subtract), [("MU", l)], T("dec4"))
        act(lambda e: e.activation(out=dec4[:], in_=dec4[:], func=AF.Exp), T("dec4"), T("dec4"))
        mub = mul[:, 1:5].unsqueeze(2).to_broadcast([4, 4, 128])
        v3 = lambda t: t[:, :].rearrange("p (c t) -> p c t", c=4)
        dve(lambda e: e.tensor_tensor(out=v3(gT1), in0=v3(gI), in1=mub, op=ALU.subtract), T("gI") + [("MU", l)], T("gv", 0))
        dve(lambda e: e.tensor_scalar(out=gT1, in0=gT1, scalar1=-0.5 * float(np.log(128.0)), scalar2=None, op0=ALU.add),
            T("gv", 0), T("gv", 0))
        act(lambda e: e.activation(out=gW[:], in_=gT1, func=AF.Exp), T("gv", 0), T("gW"))
        dve(lambda e: e.tensor_tensor(out=v3(gT2), in0=v3(gB), in1=mub, op=ALU.add), T("gv", 1) + [("MU", l)], T("gv", 1))
        act(lambda e: e.activation(out=gE[:], in_=gT2, func=AF.Exp, scale=-2.0), T("gv", 1), T("gE"))
        dve(lambda e: e.tensor_copy(out=mul[:, 0:1], in_=mul[:, 4:5]), [("MU", l)] + T("dec4") + T("gv", 0) + T("gv", 1), [("MU", l)])
        P.op("pool", lambda e: e.memset(vaug[:, :, :, 256:257], 1.0), writes=[("vaug", c) for c in range(4)])
        tl = tails[:, l * 24:(l + 1) * 24].rearrange("p (b t) -> p b t", b=8)
        for g in range(2):
            st, wt = load_kcols(Win, C_QK + g * 512, 512, cvtok(l, "in"))
            wv = kview(st, 512)
            for jj in range(4):
                blk = g * 4 + jj
                zi = blk % 2
                pk, tk = nextbank()
                for k in range(8):
                    mm(pk[:, :], wv[:, k, jj * 128:(jj + 1) * 128], xn[:, k, :], k == 0, k == 7, [wt] + T("xn", k), tk)
                P.op("pool", lambda e, blk=blk, zi=zi: e.tensor_copy(out=zb[:, zi, 0:3], in_=tl[:, blk, :]),
                     reads=[("tails", l)], writes=T("zb", zi))
                act(lambda e, pk=pk, zi=zi: e.activation(out=zb[:, zi, 3:TM + 3], in_=pk[:, :], func=AF.Copy),
                    [tk], T("zb", zi))
                P.op("pool", lambda e, blk=blk, zi=zi: e.tensor_copy(out=tl[:, blk, :], in_=zb[:, zi, TM:TM + 3]),
                     reads=T("zb", zi), writes=[("tails", l)])
                for j in range(4):
                    col = pvcol("cw", l, j, blk)
                    if j == 0:
                        P.op("dve", lambda e, zi=zi, col=col: e.tensor_scalar(
                            out=ctmp[:, zi, :], in0=zb[:, zi, 0:TM], scalar1=pv[:, col:col + 1], scalar2=None, op0=ALU.mult),
                            reads=T("zb", zi) + T("pv"), writes=T("ctmp", zi))
                    else:
                        P.op("dve", lambda e, zi=zi, col=col, j=j: e.scalar_tensor_tensor(
                            out=ctmp[:, zi, :], in0=zb[:, zi, j:TM + j], scalar=pv[:, col:col + 1], in1=ctmp[:, zi, :],
                            op0=ALU.mult, op1=ALU.add),
                            reads=T("zb", zi) + T("pv") + T("ctmp", zi), writes=T("ctmp", zi))
                colb = pvcol("cb", l, blk)
                act(lambda e, zi=zi, blk=blk, colb=colb: e.activation(out=bufA[:, blk, :], in_=ctmp[:, zi, :], func=AF.Silu,
                                                                       bias=pv[:, colb:colb + 1]),
                    T("ctmp", zi) + T("pv"), T("bufA", blk))
        for g in range(2):
            st, wt = load_kcols(Win, C_V + g * 512, 512, cvtok(l, "in"))
            wv = kview(st, 512)
            for c in range(4):
                pk, tk = nextbank()
                for k in range(8):
                    mm(pk[:, :], xn[:, k, c * 128:(c + 1) * 128], wv[:, k, :], k == 0, k == 7, [wt] + T("xn", k), tk)
                act(lambda e, pk=pk, c=c, g=g: e.activation(
                    out=vaug[:, c, 2 * g:2 * g + 2, 0:256], in_=pk[:, :].rearrange("p (h n) -> p h n", h=2), func=AF.Copy),
                    [tk], T("vaug", c))
        for g in range(2):
            st, wt = load_kcols(Win, C_O + g * 512, 512, cvtok(l, "in"))
            wv = kview(st, 512)
            for jj in range(4):
                blk = g * 4 + jj
                pk, tk = nextbank()
                for k in range(8):
                    mm(pk[:, :], wv[:, k, jj * 128:(jj + 1) * 128], xn[:, k, :], k == 0, k == 7, [wt] + T("xn", k), tk)
                act(lambda e, pk=pk, blk=blk: e.activation(out=bufB[:, blk, :], in_=pk[:, :], func=AF.Sigmoid),
                    [tk], T("bufB", blk))
                col = pvcol("mg", l, blk)
                P.op("dve", lambda e, blk=blk, col=col: e.tensor_scalar(
                    out=bufB[:, blk, :], in0=bufB[:, blk, :], scalar1=pv[:, col:col + 1], scalar2=None, op0=ALU.mult),
                    reads=T("bufB", blk) + T("pv"), writes=T("bufB", blk))
        for c in range(4):
            mm(bk6[:, c * 8:c * 8 + 4], gW[:, c * 128:(c + 1) * 128], sel[:, :].rearrange("p (h m) -> p h m", h=4)[:, :, 0],
               True, True, T("gW") + T("sel"), t6)
            mm(bk6[:, c * 8 + 4:c * 8 + 8], gE[:, c * 128:(c + 1) * 128], sel[:, :].rearrange("p (h m) -> p h m", h=4)[:, :, 0],
               True, True, T("gE") + T("sel"), t6)
        dve(lambda e: e.tensor_copy(out=tokW[:, :, :].rearrange("p c n -> p (c n)"), in_=bk6[:, 0:32]), [t6], T("tokW"))
        for hh in range(4):
            mm(bk7[:, hh * 4:hh * 4 + 4], sel[:, hh * 128:(hh + 1) * 128], dec4[:, :], True, True, T("dec4") + T("sel"), t7)
        dve(lambda e: e.tensor_copy(out=decb[:, :], in_=bk7[:, 0:16]), [t7], T("decb"))

        for c in range(4):
            pk, tk = nextbank()
            pkb = pk[:, :].bitcast(BF16)
            for hh in range(4):
                P.op("pe", lambda e, pkb=pkb, hh=hh, c=c: e.transpose(out=pkb[:, hh * 128:(hh + 1) * 128],
                                                                      in_=bufA[:, 4 + hh, c * 128:(c + 1) * 128],
                                                                      identity=ident[:]),
                     reads=T("bufA", 4 + hh) + T("ident"), writes=[tk])
            dve(lambda e, pkb=pkb, c=c: e.tensor_tensor(
                out=kp[:, c, :].rearrange("p (h d) -> p h d", h=4), in0=pkb[:, 0:512].rearrange("p (h d) -> p h d", h=4),
                in1=tokW[:, c, 0:4].unsqueeze(2).to_broadcast([128, 4, 128]), op=ALU.mult),
                [tk] + T("tokW"), T("kp", c))
        fill_rr = [0]

        def filler(i):
            which, g = ("u", i) if i < 2 else ("xq", i - 2)
            c0 = (C_U if which == "u" else C_XQ) + g * 512
            st, wt = load_kcols(Win, c0, 512, cvtok(l, "in"))
            wv = kview(st, 512)
            for jj in range(4):
                blk = g * 4 + jj
                bi_ = 6 + fill_rr[0] % 2
                fill_rr[0] += 1
                pk, tk = banks[bi_], ("bank", bi_)
                for k in range(8):
                    mm(pk[:, :], wv[:, k, jj * 128:(jj + 1) * 128], xn[:, k, :], k == 0, k == 7, [wt] + T("xn", k), tk)
                if which == "u":
                    act(lambda e, pk=pk, blk=blk: e.activation(out=bufC[:, blk, :], in_=pk[:, :], func=AF.Gelu_apprx_tanh),
                        [tk], T("bufC", blk))
                else:
                    act(lambda e, pk=pk, blk=blk: e.activation(out=xqT[:, blk, :], in_=pk[:, :], func=AF.Copy),
                        [tk], T("xqT", blk))

        for c in range(4):
            cs = slice(c * 128, (c + 1) * 128)
            pS, tS = nextbank()
            for hh in range(4):
                mm(pS[:, hh * 128:(hh + 1) * 128], bufA[:, 4 + hh, cs], bufA[:, hh, cs], True, True,
                   T("bufA", 4 + hh) + T("bufA", hh), tS)
            for hh in range(4):
                dve(lambda e, hh=hh, c=c, pS=pS: e.scalar_tensor_tensor(
                    out=ST[:, c, hh * 128:(hh + 1) * 128], in0=pS[:, hh * 128:(hh + 1) * 128], scalar=tokW[:, c, hh:hh + 1],
                    in1=maskb[:], op0=ALU.mult, op1=ALU.mult), [tS] + T("tokW") + T("maskb"), T("ST", c))
            for hh in range(4):
                act(lambda e, hh=hh, c=c: e.activation(out=Cbf[:, c, hh * 257:(hh + 1) * 257], in_=Ct[:, hh, :],
                                                       func=AF.Copy, scale=decb[:, hh * 4 + c:hh * 4 + c + 1]),
                    [("C", l)] + T("decb"), T("Cbf", c))
            for hh in range(4):
                pP, tP = nextbank()
                mm(pP[:, 0:257], kp[:, c, hh * 128:(hh + 1) * 128], vaug[:, c, hh, :], True, True,
                   T("kp", c) + T("vaug", c), tP)
                dve(lambda e, hh=hh, c=c, pP=pP: e.scalar_tensor_tensor(
                    out=Ct[:, hh, :], in0=Ct[:, hh, :], scalar=decb[:, hh * 4 + c:hh * 4 + c + 1], in1=pP[:, 0:257],
                    op0=ALU.mult, op1=ALU.add), [tP, ("C", l)] + T("decb"), [("C", l)])
        for c in range(4):
            cs = slice(c * 128, (c + 1) * 128)
            ci = c % 2
            si = c % 2
            smv = sm[:, si, :]
            pNs = []
            for hh in range(4):
                pN, tN = nextbank()
                pNs.append((pN, tN))
                mm(pN[:, 0:257], ST[:, c, hh * 128:(hh + 1) * 128], vaug[:, c, hh, :], True, False,
                   T("ST", c) + T("vaug", c), tN)
                mm(pN[:, 0:257], bufA[:, hh, cs], Cbf[:, c, hh * 257:(hh + 1) * 257], False, True,
                   T("bufA", hh) + T("Cbf", c), tN)
            pT8, tT8 = nextbank()
            pT8b = pT8[:, :].bitcast(BF16)
            filler(c)
            for hh in range(4):
                pN, tN = pNs[hh]
                dve(lambda e, pN=pN, hh=hh: e.bn_stats(out=st6[:, hh, :], in_=pN[:, 0:256]), [tN], T("st6", hh))
                dve(lambda e, pN=pN, hh=hh, smv=smv: e.tensor_copy(out=smv[:, hh:hh + 1], in_=pN[:, 256:257]),
                    [tN], T("sm", si))
            for hh in range(4):
                dve(lambda e, hh=hh: e.bn_aggr(out=mv[:, hh, :], in_=st6[:, hh, :]), T("st6", hh), T("mv", hh))
            dve(lambda e, smv=smv: e.tensor_tensor(out=smv[:, 4:8], in0=smv[:, 0:4], in1=smv[:, 0:4], op=ALU.mult),
                T("sm", si), T("sm", si))
            dve(lambda e, smv=smv, c=c: e.tensor_tensor(out=smv[:, 4:8], in0=smv[:, 4:8], in1=tokW[:, c, 4:8], op=ALU.max),
                T("sm", si) + T("tokW"), T("sm", si))
            dve(lambda e, smv=smv: e.scalar_tensor_tensor(out=smv[:, 8:12], in0=smv[:, 4:8], scalar=EPS, in1=mv[:, :, 1],
                                                          op0=ALU.mult, op1=ALU.add),
                T("sm", si) + [("mv", g4) for g4 in range(4)], T("sm", si))
            act(lambda e, smv=smv: e.activation(out=smv[:, 12:16], in_=smv[:, 8:12], func=AF.Sqrt), T("sm", si), T("sm", si))
            dve(lambda e, smv=smv: e.reciprocal(out=smv[:, 12:16], in_=smv[:, 12:16]), T("sm", si), T("sm", si))
            for hh in range(4):
                pN, tN = pNs[hh]
                dve(lambda e, pN=pN, hh=hh, ci=ci, smv=smv: e.tensor_scalar(
                    out=hn[:, ci, hh * 256:(hh + 1) * 256], in0=pN[:, 0:256], scalar1=mv[:, hh, 0:1],
                    scalar2=smv[:, 12 + hh:13 + hh], op0=ALU.subtract, op1=ALU.mult),
                    [tN] + T("mv", hh) + T("sm", si), T("hn", ci))
                for i2 in range(2):
                    P.op("pe", lambda e, hh=hh, i2=i2, ci=ci, pT8b=pT8b: e.transpose(
                        out=pT8b[:, (2 * hh + i2) * 128:(2 * hh + i2 + 1) * 128],
                        in_=hn[:, ci, hh * 256 + i2 * 128:hh * 256 + (i2 + 1) * 128], identity=ident[:]),
                        reads=T("hn", ci) + T("ident"), writes=[tT8])
            dve(lambda e, pT8b=pT8b, cs=cs: e.tensor_tensor(
                out=ymT[:, :, cs], in0=pT8b[:, 0:1024].rearrange("p (k t) -> p k t", k=8), in1=bufB[:, :, cs], op=ALU.mult),
                [tT8] + [("bufB", k) for k in range(8)], [("ymT", k) for k in range(8)])

        def merge_group(b, ysrc, ytok, first, last, mp):
            Wb = WBR.ap()[l, b]
            if True:
                stw, wtw = load_kcols(Wb, mp * 256, 256, cvtok(l, "br") + cvtok(l, "in"),
                                      extra=(C_G + b * 1024 + mp * 256, 256), extra_W=Win)
                wtg2 = wtw
                ww = kview(stw, 256)
                wgv = kview(stw, 256, off=2048)
                for mi in range(2):
                    m = mp * 2 + mi
                    pp, tp = nextbank()
                    pg, tg = nextbank()
                    for k in range(8):
                        mm(pg[:, :], wgv[:, k, mi * 128:(mi + 1) * 128], xn[:, k, :], k == 0, k == 7, [wtg2] + T("xn", k), tg)
                    for k in range(8):
                        mm(pp[:, :], ww[:, k, mi * 128:(mi + 1) * 128], ysrc[:, k, :], k == 0, k == 7,
                           [wtw] + [(ytok, k)], tp)
                    si = m % 2
                    act(lambda e, pg=pg, si=si: e.activation(out=sig[:, si, :], in_=pg[:, :], func=AF.Sigmoid),
                        [tg], T("sig", si))
                    if first:
                        dve(lambda e, pp=pp, si=si, m=m: e.tensor_tensor(out=fT[:, m, :], in0=pp[:, :], in1=sig[:, si, :],
                                                                         op=ALU.mult), [tp] + T("sig", si), T("fT", m))
                    else:
                        mi2 = m % 2
                        dve(lambda e, pp=pp, si=si, mi2=mi2: e.tensor_tensor(out=mtmp[:, mi2, :], in0=pp[:, :], in1=sig[:, si, :],
                                                                           op=ALU.mult), [tp] + T("sig", si), T("ctmp", mi2))
                        if not last:
                            P.op("dve", lambda e, m=m, mi2=mi2: e.tensor_tensor(out=fT[:, m, :], in0=fT[:, m, :],
                                                                                in1=mtmp[:, mi2, :], op=ALU.add),
                                 reads=T("fT", m) + T("ctmp", mi2), writes=T("fT", m))
                        else:
                            P.op("dve", lambda e, m=m, mi2=mi2: e.tensor_tensor(out=bufC[:, m, :], in0=fT[:, m, :],
                                                                                in1=mtmp[:, mi2, :], op=ALU.add),
                                 reads=T("fT", m) + T("ctmp", mi2), writes=T("bufC", m))


        lg = rowp[:, (l * 2) * D:(l * 2 + 1) * D]
        lb = rowp[:, (l * 2 + 1) * D:(l * 2 + 2) * D]
        gv4 = gv[:, :, :].rearrange("p a (b n) -> p (a b) n", b=2)
        for g in range(2):
            st, wt = load_kcols(Win, C_SV + g * 512, 512, cvtok(l, "in"))
            wv = kview(st, 512)
            si = g % 2
            smv = sm[:, si, :]
            for c in range(4):
                pk, tk = nextbank()
                for k in range(8):
                    mm(pk[:, :], xn[:, k, c * 128:(c + 1) * 128], wv[:, k, :], k == 0, k == 7, [wt] + T("xn", k), tk)
                act(lambda e, pk=pk, c=c: e.activation(out=gv4[:, c, :], in_=pk[:, :], func=AF.Gelu_apprx_tanh),
                    [tk], T("gv", c // 2))
                for g2 in range(2):
                    q = c * 2 + g2
                    dve(lambda e, c=c, g2=g2, q=q: e.bn_stats(out=st8[:, q, :], in_=gv4[:, c, g2 * 256:(g2 + 1) * 256]),
                        T("gv", c // 2), T("st8", q))
                    dve(lambda e, q=q: e.bn_aggr(out=mv8[:, q, :], in_=st8[:, q, :]), T("st8", q), T("mv8", q))
            act(lambda e, smv=smv: e.activation(out=smv[:, 0:8], in_=mv8[:, :, 1], func=AF.Sqrt, bias=EPS),
                [("mv8", q) for q in range(8)], T("sm", si))
            dve(lambda e, smv=smv: e.reciprocal(out=smv[:, 8:16], in_=smv[:, 0:8]), T("sm", si), T("sm", si))
            c0 = g * 512
            for c in range(4):
                for g2 in range(2):
                    q = c * 2 + g2
                    dve(lambda e, g2=g2, c=c, q=q, smv=smv, c0=c0: e.tensor_scalar(
                        out=vn[:, c, c0 + g2 * 256:c0 + (g2 + 1) * 256], in0=gv4[:, c, g2 * 256:(g2 + 1) * 256],
                        scalar1=mv8[:, q, 0:1], scalar2=smv[:, 8 + q:9 + q], op0=ALU.subtract, op1=ALU.mult),
                        T("gv", c // 2) + T("mv8", q) + T("sm", si), T("vn", c))
                dve(lambda e, c=c, c0=c0: e.tensor_tensor(out=vn[:, c, c0:c0 + 512], in0=vn[:, c, c0:c0 + 512],
                                                         in1=lg[:, c0:c0 + 512], op=ALU.mult),
                    T("vn", c) + T("rowp"), T("vn", c))
                dve(lambda e, c=c, c0=c0: e.tensor_tensor(out=vn[:, c, c0:c0 + 512], in0=vn[:, c, c0:c0 + 512],
                                                         in1=lb[:, c0:c0 + 512], op=ALU.add),
                    T("vn", c) + T("rowp"), T("vn", c))
        for c in range(4):
            cs = slice(c * 128, (c + 1) * 128)
            pa2 = [nextbank(), nextbank()]
            for j in range(8):
                g = j // 2
                pk, tk = pa2[j // 4]
                osl = slice((j % 4) * 128, (j % 4 + 1) * 128)
                wo = (l * 4 + g) * 128
                mm(pk[:, osl], vn[:, c, j * 128:(j + 1) * 128], wsT[:, wo:wo + 128], True, False, T("vn", c) + T("wsT"), tk)
                mm(pk[:, osl], onesb[0:1, :], bshi[0:1, wo:wo + 128], False, False, T("onesb") + T("bshi"), tk)
                mm(pk[:, osl], onesb[0:1, :], bslo[0:1, wo:wo + 128], False, True, T("onesb") + T("bslo"), tk)
            for hf in range(2):
                pk, tk = pa2[hf]
                dve(lambda e, pk=pk, hf=hf, cs=cs: e.tensor_tensor(
                    out=bufB[:, 4 * hf:4 * hf + 4, cs], in0=pk[:, :].rearrange("p (k t) -> p k t", k=4),
                    in1=bufC[:, 4 * hf:4 * hf + 4, cs], op=ALU.mult),
                    [tk] + [("bufC", k) for k in range(4 * hf, 4 * hf + 4)], [("bufB", k) for k in range(4 * hf, 4 * hf + 4)])
        def m5_s1(c):
            cs = slice(c * 128, (c + 1) * 128)
            pi = c % 2
            psc = [nextbank(), nextbank()]
            for hh in range(4):
                pk, tk = psc[hh // 2]
                osl = slice((hh % 2) * 256, (hh % 2 + 1) * 256)
                for i2 in range(2):
                    j = 2 * hh + i2
                    o0 = (l * 8 + j) * 256
                    mm(pk[:, osl], xqT[:, j, cs], kmT[:, o0:o0 + 256], i2 == 0, i2 == 1, T("xqT", j) + T("kmT", l), tk)
            si = c % 2
            smv = sm[:, si, :]
            for hf in range(2):
                pk, tk = psc[hf]
                dve(lambda e, pk=pk, hf=hf, smv=smv: e.tensor_reduce(
                    out=smv[:, 2 * hf:2 * hf + 2], in_=pk[:, :].rearrange("p (h m) -> p h m", h=2), axis=AX.X, op=ALU.max),
                    [tk], T("sm", si))
            dve(lambda e, smv=smv: e.tensor_scalar(out=smv[:, 4:8], in0=smv[:, 0:4], scalar1=-1.0 / 16.0, scalar2=None,
                                                   op0=ALU.mult), T("sm", si), T("sm", si))
            for hh in range(4):
                pk, tk = psc[hh // 2]
                osl = slice((hh % 2) * 256, (hh % 2 + 1) * 256)
                act(lambda e, pk=pk, osl=osl, hh=hh, pi=pi, smv=smv: e.activation(
                    out=pb[:, pi, hh * 256:(hh + 1) * 256], in_=pk[:, osl], func=AF.Exp, bias=smv[:, 4 + hh:5 + hh],
                    scale=1.0 / 16.0, accum_out=smv[:, 8 + hh:9 + hh]), [tk] + T("sm", si), T("hn", pi) + T("sm", si))
            dve(lambda e, smv=smv: e.reciprocal(out=smv[:, 12:16], in_=smv[:, 8:12]), T("sm", si), T("sm", si))
            dve(lambda e, pi=pi, smv=smv: e.tensor_tensor(
                out=pb[:, pi, :].rearrange("p (h m) -> p h m", h=4), in0=pb[:, pi, :].rearrange("p (h m) -> p h m", h=4),
                in1=smv[:, 12:16].unsqueeze(2).to_broadcast([128, 4, 256]), op=ALU.mult),
                T("hn", pi) + T("sm", si), T("hn", pi))
        def m5_s2(c):
            cs = slice(c * 128, (c + 1) * 128)
            pi = c % 2
            pk8, tk8 = nextbank()
            pk8b = pk8[:, :].bitcast(BF16)
            for hh in range(4):
                for mc in range(2):
                    q8 = hh * 2 + mc
                    P.op("pe", lambda e, pk8b=pk8b, q8=q8, pi=pi, hh=hh, mc=mc: e.transpose(
                        out=pk8b[:, q8 * 128:(q8 + 1) * 128], in_=pb[:, pi, hh * 256 + mc * 128:hh * 256 + (mc + 1) * 128],
                        identity=ident[:]), reads=T("hn", pi) + T("ident"), writes=[tk8])
            act(lambda e, pk8b=pk8b, pi=pi: e.activation(out=pT[:, pi, :], in_=pk8b[:, 0:1024], func=AF.Copy),
                [tk8], T("pT", pi))
            pyx = [nextbank(), nextbank()]
            for j in range(8):
                hh = j // 2
                pk, tk = pyx[j // 4]
                osl = slice((j % 4) * 128, (j % 4 + 1) * 128)
                for mc in range(2):
                    o0 = (l * 2 + mc) * 1024 + j * 128
                    q8 = hh * 2 + mc
                    mm(pk[:, osl], vm[:, o0:o0 + 128], pT[:, pi, q8 * 128:(q8 + 1) * 128], mc == 0, mc == 1,
                       T("vm", l) + T("pT", pi), tk)
            for hf in range(2):
                pk, tk = pyx[hf]
                act(lambda e, pk=pk, hf=hf, cs=cs: e.activation(
                    out=bufA[:, 4 * hf:4 * hf + 4, cs], in_=pk[:, :].rearrange("p (k t) -> p k t", k=4), func=AF.Copy),
                    [tk], [("bufA", k) for k in range(4 * hf, 4 * hf + 4)])

        fence()
        for c in range(4):
            m5_s1(c)
            merge_group(0, ymT, "ymT", True, False, c)
            merge_group(1, bufB, "bufB", False, False, c)
            m5_s2(c)
        for mp in range(4):
            merge_group(2, bufA, "bufA", False, True, mp)
        Wo = WOUT.ap()[l]
        for g in range(2):
            st, wt = load_kcols(Wo, g * 512, 512, cvtok(l, "out"))
            wv = kview(st, 512)
            for jj in range(4):
                m = g * 4 + jj
                pk, tk = nextbank()
                for k in range(8):
                    mm(pk[:, :], wv[:, k, jj * 128:(jj + 1) * 128], bufC[:, k, :], k == 0, k == 7, [wt] + T("bufC", k), tk)
                act(lambda e, pk=pk, m=m: e.activation(out=fT[:, m, :], in_=pk[:, :], func=AF.Copy), [tk], T("fT", m))
        postnorm_residual(l, 3, False)

    setup_kv()
    for t in range(NT):
        ts = slice(t * TM, (t + 1) * TM)
        for k in range(8):
            P.dma("sp", h[:, k, :], xT_d.ap()[k * 128:(k + 1) * 128, ts], writes=T("h", k), stream="ld_h%d" % k)
        for l in range(L):
            ffn(l, 0)
            if cfg.stages == "ffn1":
                continue
            fence()
            mixer(l)
            fence()
            if cfg.stages == "mix":
                continue
            ffn(l, 1)
        for k in range(8):
            P.dma("sp", yT_d.ap()[k * 128:(k + 1) * 128, ts], h[:, k, :], reads=T("h", k), writes=[("yT", k)], stream="st_y%d" % k)
    pump(len(jobs))
    P.barrier("sp", [("yT", k) for k in range(8)])
    P.emit()
    return nc, P


def prep_shared(inp, cfg):
    L = cfg.L
    f = lambda a: np.ascontiguousarray(np.asarray(a, dtype=np.float32))
    ng = f(inp["norm_g"])[:L]
    cw = f(inp["conv_w"])[:L]
    cb = f(inp["conv_b"])[:L]
    mg = f(inp["mlstm_norm_g"])[:L]
    pv = np.concatenate([
        ng.reshape(L, 7, 8, 128).transpose(3, 0, 1, 2).reshape(128, L * 56),
        cw.reshape(L, 4, 8, 128).transpose(3, 0, 1, 2).reshape(128, L * 32),
        cb.reshape(L, 8, 128).transpose(2, 0, 1).reshape(128, L * 8),
        mg.reshape(L, 8, 128).transpose(2, 0, 1).reshape(128, L * 8)], axis=1)
    lg = f(inp["sgu_ln_g"])[:L]
    lb = f(inp["sgu_ln_b"])[:L]
    rowp = np.stack([lg, lb], axis=1).reshape(1, L * 2 * D)
    rowp = np.ascontiguousarray(np.broadcast_to(rowp, (128, L * 2 * D)))
    ws = f(inp["sgu_w_s"])[:L]
    wsT = np.ascontiguousarray(ws.transpose(3, 0, 1, 2).reshape(128, L * 4 * 128))
    bs = f(inp["sgu_b_s"])[:L].reshape(1, L * 512)
    bif = np.stack([f(inp["b_i"])[:L], f(inp["b_f"])[:L]], axis=1)
    bif = np.ascontiguousarray(bif.transpose(2, 0, 1).reshape(4, 2 * L))
    ident = np.eye(128, dtype=np.float32).astype(ml_dtypes.bfloat16)
    maskle = np.triu(np.ones((128, 128), dtype=np.float32)).astype(ml_dtypes.bfloat16)
    sel = np.zeros((4, 4, 128), dtype=np.float32)
    for hh in range(4):
        sel[hh, hh, :] = 1.0
    sel = sel.reshape(4, 512)
    return {
        "w_gu": f(inp["w_ffn_gu"])[:L], "w_dn": f(inp["w_ffn_down"])[:L], "w_in": f(inp["w_in"])[:L],
        "w_kv": f(inp["w_mkv"])[:L], "w_br": f(inp["w_branch"])[:L], "w_out": f(inp["w_out"])[:L],
        "pvec": np.ascontiguousarray(pv), "rowp": rowp, "wsT": wsT, "bs": np.ascontiguousarray(bs), "bif": bif,
        "ident": ident, "maskle": maskle, "sel": sel,
    }


_CACHE = {}


def run(inp, cfg, n_cores, trace=False):
    key = (cfg.S, cfg.L, cfg.stages, cfg.preconvert)
    if key not in _CACHE:
        _CACHE[key] = build_program(cfg)
    nc, P = _CACHE[key]
    shared = prep_shared(inp, cfg)
    x = np.asarray(inp["x"], dtype=np.float32)
    mem = np.asarray(inp["mem"], dtype=np.float32)
    in_maps = []
    for b in range(n_cores):
        m = dict(shared)
        m["xT"] = np.ascontiguousarray(x[b, :cfg.S].T)
        m["memT"] = np.ascontiguousarray(mem[b].T)
        in_maps.append(m)
    res = run_bass_kernel_spmd(nc, in_maps, core_ids=list(range(n_cores)), trace=trace)
    out = np.stack([np.ascontiguousarray(r["yT"].T) for r in res.results], axis=0)
    return out.astype(np.float32), res


def kernel(x, mem, norm_g, w_ffn_gu, w_ffn_down, w_in, conv_w, conv_b, b_i, b_f, mlstm_norm_g, sgu_ln_g, sgu_ln_b,
           sgu_w_s, sgu_b_s, w_mkv, w_branch, w_out):
    inp = dict(x=x, mem=mem, norm_g=norm_g, w_ffn_gu=w_ffn_gu, w_ffn_down=w_ffn_down, w_in=w_in, conv_w=conv_w,
               conv_b=conv_b, b_i=b_i, b_f=b_f, mlstm_norm_g=mlstm_norm_g, sgu_ln_g=sgu_ln_g, sgu_ln_b=sgu_ln_b,
               sgu_w_s=sgu_w_s, sgu_b_s=sgu_b_s, w_mkv=w_mkv, w_branch=w_branch, w_out=w_out)
    cfg = Cfg(S=4096, L=2, stages="all", preconvert=True)
    out, _ = run(inp, cfg, 8)
    return out
```

```python
import contextlib
import numpy as np
import ml_dtypes
import concourse.bass as bass
import concourse.mybir as mybir
from concourse.bass_utils import run_bass_kernel_spmd

F32 = mybir.dt.float32
BF16 = mybir.dt.bfloat16
AF = mybir.ActivationFunctionType
ALU = mybir.AluOpType
AX = mybir.AxisListType

D = 1024
DFF = 2816
MEM = 256
WIN = 9224
TM = 512
C_QK, C_V, C_O, C_I, C_F, C_U, C_SV, C_XQ, C_G = 0, 1024, 2048, 3072, 3076, 3080, 4104, 5128, 6152
EPS = 1e-6
NSLOT = 3
SLOTW = 4096

ENGINES = ("pe", "act", "dve", "pool", "sp")


class Op:
    __slots__ = ("eng", "fn", "deps", "is_dma", "stream", "signaled", "sem", "val", "clock")

    def __init__(self, eng, fn, is_dma=False, stream=None):
        self.eng = eng
        self.fn = fn
        self.deps = []
        self.is_dma = is_dma
        self.stream = stream
        self.signaled = False
        self.sem = None
        self.val = 0
        self.clock = None


class Prog:
    def __init__(self, nc):
        self.nc = nc
        self.ops = []
        self.by_eng = {e: [] for e in ENGINES}
        self.last_writer = {}
        self.readers = {}

    def _add_dep(self, op, d):
        if d is None or d is op:
            return
        if d.is_dma and op.is_dma and d.stream == op.stream:
            return
        if d not in op.deps:
            op.deps.append(d)

    def op(self, eng, fn, reads=(), writes=(), dma=False, stream=None):
        o = Op(eng, fn, is_dma=dma, stream=stream)
        for t in reads:
            w = self.last_writer.get(t)
            if w is not None and w.eng == eng and eng == "pe" and not w.is_dma and not dma:
                continue
            self._add_dep(o, w)
        for t in writes:
            w = self.last_writer.get(t)
            if w is not None and not (w.eng == eng and not w.is_dma and not dma):
                self._add_dep(o, w)
            for r in self.readers.get(t, ()):
                if not (r.eng == eng and not r.is_dma and not dma):
                    self._add_dep(o, r)
        for t in reads:
            rl = self.readers.setdefault(t, [])
            if not dma:
                for i2 in range(len(rl)):
                    if rl[i2].eng == eng and not rl[i2].is_dma:
                        rl[i2] = o
                        break
                else:
                    rl.append(o)
            else:
                rl.append(o)
        for t in writes:
            self.last_writer[t] = o
            self.readers[t] = []
        self.ops.append(o)
        self.by_eng[eng].append(o)
        return o

    def dma(self, q, out, in_, reads=(), writes=(), stream=None):
        return self.op(q, lambda e: e.dma_start(out=out, in_=in_), reads=reads, writes=writes,
                       dma=True, stream=stream)

    def barrier(self, eng, toks):
        return self.op(eng, None, reads=toks)

    def emit(self):
        nc = self.nc
        for o in self.ops:
            for d in o.deps:
                d.signaled = True
        stack = contextlib.ExitStack()
        eng_sem = {e: stack.enter_context(nc.semaphore("s_" + e)) for e in ENGINES}
        stream_sem = {}
        for o in self.ops:
            if o.is_dma and o.stream not in stream_sem:
                stream_sem[o.stream] = stack.enter_context(nc.semaphore("d_" + o.stream))
        cnt = {e: 0 for e in ENGINES}
        scnt = {s: 0 for s in stream_sem}
        for o in self.ops:
            if o.is_dma:
                scnt[o.stream] += 1
                o.sem = stream_sem[o.stream]
                o.val = 16 * scnt[o.stream]
            elif o.signaled and o.fn is not None:
                cnt[o.eng] += 1
                o.sem = eng_sem[o.eng]
                o.val = cnt[o.eng]
        eng_clock = {e: {} for e in ENGINES}
        waits = {}
        nw = 0
        for o in self.ops:
            ck = eng_clock[o.eng]
            w = []
            for d in o.deps:
                key = id(d.sem)
                if ck.get(key, 0) >= d.val:
                    continue
                w.append((d.sem, d.val))
                for k2, v2 in d.clock.items():
                    if ck.get(k2, 0) < v2:
                        ck[k2] = v2
                ck[key] = d.val
            waits[id(o)] = w
            nw += len(w)
            o.clock = dict(ck) if o.signaled or o.is_dma else None
        self.n_waits = nw
        self.sem_counts = dict(cnt)
        handles = {"pe": "tensor", "act": "scalar", "dve": "vector", "pool": "gpsimd", "sp": "sync"}
        with nc.Block() as block:
            for e in ENGINES:
                ops = self.by_eng[e]
                if not ops:
                    continue

                def body(eh, ops=ops):
                    for o in ops:
                        for (s, v) in waits[id(o)]:
                            eh.wait_ge(s, v)
                        if o.fn is None:
                            continue
                        ins = o.fn(eh)
                        if o.is_dma:
                            ins.then_inc(o.sem, 16)
                        elif o.signaled:
                            ins.then_inc(o.sem, 1)
                getattr(block, handles[e])(body)
        stack.close()


class Cfg:
    def __init__(self, S=4096, L=2, stages="all", preconvert=True):
        self.S = S
        self.L = L
        self.stages = stages
        self.preconvert = preconvert


def build_program(cfg):
    S, L = cfg.S, cfg.L
    NT = S // TM
    nc = bass.Bass("TRN2", target_bir_lowering=False)
    P = Prog(nc)

    def dram_in(name, shape, dt=F32):
        return nc.dram_tensor(name, list(shape), dt, kind="ExternalInput")

    xT_d = dram_in("xT", [D, S])
    memT_d = dram_in("memT", [D, MEM])
    wgu_d = dram_in("w_gu", [L, 2, D, 2 * DFF])
    wdn_d = dram_in("w_dn", [L, 2, DFF, D])
    win_d = dram_in("w_in", [L, D, WIN])
    wkv_d = dram_in("w_kv", [L, D, 2 * D])
    wbr_d = dram_in("w_br", [L, 3, D, D])
    wout_d = dram_in("w_out", [L, D, D])
    NPV = 104 * L
    pv_d = dram_in("pvec", [128, NPV])
    rowp_d = dram_in("rowp", [128, L * 2 * D])
    wsT_d = dram_in("wsT", [128, L * 4 * 128])
    bs_d = dram_in("bs", [1, L * 512])
    bif_d = dram_in("bif", [4, 2 * L])
    ident_d = dram_in("ident", [128, 128], BF16)
    mask_d = dram_in("maskle", [128, 128], BF16)
    sel_d = dram_in("sel", [4, 512])
    yT_d = nc.dram_tensor("yT", [D, S], F32, kind="ExternalOutput")

    if cfg.preconvert:
        sc = lambda name, shape: nc.dram_tensor(name, list(shape), BF16, kind="Internal")
        wgu_b = sc("wgu_b", [L, 2, D, 2 * DFF])
        wdn_b = sc("wdn_b", [L, 2, DFF, D])
        win_b = sc("win_b", [L, D, WIN])
        wkv_b = sc("wkv_b", [L, D, 2 * D])
        wbr_b = sc("wbr_b", [L, 3, D, D])
        wout_b = sc("wout_b", [L, D, D])

    def sb(name, shape, dt):
        return nc.alloc_sbuf_tensor("sb_" + name, list(shape), dt)

    ident = sb("ident", [128, 128], BF16)
    onesb = sb("onesb", [128, 128], BF16)
    maskb = sb("maskb", [128, 128], BF16)
    sel = sb("sel", [4, 512], F32)
    pv = sb("pv", [128, NPV], F32)
    pvh = sb("pvh", [128, NPV], F32)
    rowp = sb("rowp", [128, L * 2 * D], BF16)
    wsT = sb("wsT", [128, L * 4 * 128], BF16)
    bshi = sb("bshi", [1, L * 512], BF16)
    bslo = sb("bslo", [1, L * 512], BF16)
    bif = sb("bif", [4, 2 * L], F32)
    ones4 = sb("ones4", [4, 512], F32)
    Cst = sb("Cst", [128, L * 4 * 257], F32)
    tails = sb("tails", [128, L * 8 * 3], F32)
    Bprev = sb("Bprev", [4, L], F32)
    MU = sb("MU", [4, L * 5], F32)
    kmT = sb("kmT", [128, L * 8 * 256], BF16)
    vm = sb("vm", [128, L * 2 * 1024], BF16)
    h = sb("h", [128, 8, TM], F32)
    xn = sb("xn", [128, 8, TM], BF16)
    sq = sb("sq", [128, 4, TM], BF16)
    rstd = sb("rstd", [128, TM], F32)
    wslot = [sb("wslot%d" % i, [128, SLOTW], BF16) for i in range(NSLOT)]
    a012 = sb("a012", [128, 12288], BF16)
    bufA = a012[:, 0:4096].rearrange("p (k t) -> p k t", k=8)
    bufB = a012[:, 4096:8192].rearrange("p (k t) -> p k t", k=8)
    bufC = a012[:, 8192:12288].rearrange("p (k t) -> p k t", k=8)
    hid = a012[:, 0:22 * TM].rearrange("p (k t) -> p k t", k=22)
    ymT_t = sb("ymT", [128, 8 * TM], BF16)
    ymT = ymT_t[:, :].rearrange("p (k t) -> p k t", k=8)
    sil = ymT_t[:, 0:4 * TM].bitcast(F32).rearrange("p (k t) -> p k t", k=2)
    xqT = sb("xqT", [128, 8, TM], BF16)
    a5 = sb("a5", [128, 10320], BF16)
    vn = a5[:, 0:4096].rearrange("p (c n) -> p c n", c=4)
    kp = a5[:, 4096:6144].rearrange("p (c n) -> p c n", c=4)
    vaug = a5[:, 6144:6144 + 4112].rearrange("p (c h n) -> p c h n", c=4, h=4)
    fT = a5[:, 0:8192].bitcast(F32).rearrange("p (k t) -> p k t", k=8)
    gv = sb("gv", [128, 2, 1024], F32)
    zb = sb("zb", [128, 2, TM + 3], F32)
    ctmp = sb("ctmp", [128, 2, TM], F32)
    mtmp = ctmp
    sig = sb("sig", [128, 2, TM], BF16)
    Cbf = sb("Cbf", [128, 4, 4 * 257], BF16)
    ST = sb("ST", [128, 4, 512], BF16)
    hn = sb("hn", [128, 2, 1024], BF16)
    pb = hn
    pT = sb("pT", [128, 2, 1024], BF16)
    gI = sb("gI", [4, TM], F32)
    gF = gv[0:4, 0, 0:TM]
    gT1 = gv[0:4, 0, TM:2 * TM]
    gT2 = gv[0:4, 1, 0:TM]
    gB = gv[0:4, 1, TM:2 * TM]
    gW = sb("gW", [4, TM], F32)
    gE = sb("gE", [4, TM], F32)
    mx4 = sb("mx4", [4, 4], F32)
    dec4 = sb("dec4", [4, 4], F32)
    decb = sb("decb", [128, 16], F32)
    tokW = sb("tokW", [128, 4, 8], F32)
    st6 = sb("st6", [128, 4, 6], F32)
    st8 = sb("st8", [128, 8, 6], F32)
    mv8 = sb("mv8", [128, 8, 2], F32)
    mv = sb("mv", [128, 4, 2], F32)
    sm = sb("sm", [128, 2, 16], F32)
    dummy = sb("dummy", [1, 8], F32)
    epsb = sb("epsb", [128, 1], F32)

    banks = [nc.alloc_psum_tensor("bank%d" % i, [128, 512], F32) for i in range(8)]
    bank_rr = [0]

    def nextbank(lo=0, hi=6):
        i = lo + bank_rr[0] % (hi - lo)
        bank_rr[0] += 1
        return banks[i], ("bank", i)

    def T(name, k=None, c=None):
        if k is None:
            return [(name,)]
        if c is None:
            return [(name, k)]
        return [(name, k, c)]

    ARENA_TOKS = []

    def reg_arena(toks):
        ARENA_TOKS.extend(toks)
        return toks

    for k in range(8):
        reg_arena([("bufA", k), ("bufB", k), ("bufC", k), ("ymT", k), ("fT", k)])
    for j in range(22):
        reg_arena([("hid", j)])
    reg_arena([("sil", 0), ("sil", 1)])
    for c in range(4):
        reg_arena([("vn", c), ("kp", c), ("vaug", c)])

    def fence():
        P.op("pool", lambda e: e.memset(dummy[:, 0:1], 0.0), writes=list(ARENA_TOKS))

    def pvcol(kind, l, a=0, b=0):
        if kind == "ng":
            return (l * 7 + a) * 8 + b
        if kind == "cw":
            return L * 56 + (l * 4 + a) * 8 + b
        if kind == "cb":
            return L * 88 + l * 8 + a
        if kind == "mg":
            return L * 96 + l * 8 + a
        raise ValueError

    P.dma("sp", ident[:], ident_d.ap(), writes=T("ident"), stream="c0")
    P.dma("sp", maskb[:], mask_d.ap(), writes=T("maskb"), stream="c2")
    P.dma("sp", sel[:], sel_d.ap(), writes=T("sel"), stream="c3")
    P.dma("sp", pv[:], pv_d.ap(), writes=T("pv"), stream="c4")
    P.dma("sp", bif[:], bif_d.ap(), writes=T("bif"), stream="c5")
    P.dma("pool", rowp[:], rowp_d.ap(), writes=T("rowp"), stream="c1")
    gvflat = gv[:, :, :].rearrange("p a b -> p (a b)")
    bsf = gv[0:1, 1, 0:L * 512]
    P.dma("sp", bsf, bs_d.ap(), writes=T("bsf") + T("gv", 1), stream="c6")
    P.dma("sp", gvflat[:, 0:L * 512], wsT_d.ap(), writes=T("gv", 0), stream="c7")
    for l in range(L):
        for g in range(4):
            o0 = (l * 4 + g) * 128
            P.op("dve", lambda e, o0=o0: e.tensor_tensor(out=wsT[:, o0:o0 + 128], in0=gvflat[:, o0:o0 + 128],
                                                         in1=maskb[:], op=ALU.mult),
                 reads=T("gv", 0) + T("maskb"), writes=T("wsT"))
    P.op("dve", lambda e: e.memset(onesb[:], 1.0), writes=T("onesb"))
    P.op("dve", lambda e: e.memset(epsb[:], EPS), writes=T("epsb"))
    P.op("dve", lambda e: e.memset(ones4[:], 1.0), writes=T("ones4"))
    P.op("dve", lambda e: e.memset(Cst[:], 0.0), writes=[("C", l) for l in range(L)])
    P.op("dve", lambda e: e.memset(tails[:], 0.0), writes=[("tails", l) for l in range(L)])
    P.op("dve", lambda e: e.memset(Bprev[:], 0.0), writes=[("Bprev", l) for l in range(L)])
    P.op("dve", lambda e: e.memset(MU[:], -1e30), writes=[("MU", l) for l in range(L)])
    P.op("dve", lambda e: e.tensor_scalar(out=pvh[:], in0=pv[:], scalar1=0.5, scalar2=None, op0=ALU.mult),
         reads=T("pv"), writes=T("pvh"))
    P.op("dve", lambda e: e.tensor_copy(out=bshi[:], in_=bsf), reads=T("bsf"), writes=T("bshi"))
    P.op("dve", lambda e: e.tensor_tensor(out=bslo[:], in0=bsf, in1=bshi[:], op=ALU.subtract),
         reads=T("bsf") + T("bshi"), writes=T("bslo") + T("gv", 1))

    jobs = []
    job_last = {}
    jp = [0]

    def cvt(dst, src, tok, stream):
        job_last[tok] = len(jobs)
        jobs.append((dst, src, tok, stream))

    def pump(n):
        while n > 0 and jp[0] < len(jobs):
            dst, src, tok, stream = jobs[jp[0]]
            P.dma("pool", dst, src, writes=[tok], stream=stream)
            jp[0] += 1
            n -= 1

    def ensure(tok):
        if tok in job_last:
            while jp[0] <= job_last[tok]:
                pump(1)

    if cfg.preconvert:
        for l in range(L):
            for k in range(8):
                cvt(wkv_b.ap()[l, k * 128:(k + 1) * 128, :], wkv_d.ap()[l, k * 128:(k + 1) * 128, :],
                    ("cv", l, "kv"), "cv%dkv" % l)
        for l in range(L):
            for f in range(2):
                if f == 1:
                    for k in range(8):
                        for hf in range(4):
                            c0 = hf * 2306
                            cvt(win_b.ap()[l, k * 128:(k + 1) * 128, c0:c0 + 2306],
                                win_d.ap()[l, k * 128:(k + 1) * 128, c0:c0 + 2306], ("cv", l, "in"), "cv%din" % l)
                    for b in range(3):
                        for k in range(8):
                            cvt(wbr_b.ap()[l, b, k * 128:(k + 1) * 128, :], wbr_d.ap()[l, b, k * 128:(k + 1) * 128, :],
                                ("cv", l, "br"), "cv%dbr" % l)
                    for k in range(8):
                        cvt(wout_b.ap()[l, k * 128:(k + 1) * 128, :], wout_d.ap()[l, k * 128:(k + 1) * 128, :],
                            ("cv", l, "out"), "cv%dout" % l)
                for k in range(8):
                    for hf in range(2):
                        cvt(wgu_b.ap()[l, f, k * 128:(k + 1) * 128, hf * DFF:(hf + 1) * DFF],
                            wgu_d.ap()[l, f, k * 128:(k + 1) * 128, hf * DFF:(hf + 1) * DFF],
                            ("cv", l, "gu", f), "cv%dgu%d" % (l, f))
                for j in range(22):
                    cvt(wdn_b.ap()[l, f, j * 128:(j + 1) * 128, :], wdn_d.ap()[l, f, j * 128:(j + 1) * 128, :],
                        ("cv", l, "dn", f), "cv%ddn%d" % (l, f))
        WGU, WDN, WINB, WKV, WBR, WOUT = wgu_b, wdn_b, win_b, wkv_b, wbr_b, wout_b
        wq = "sp"
    else:
        WGU, WDN, WINB, WKV, WBR, WOUT = wgu_d, wdn_d, win_d, wkv_d, wbr_d, wout_d
        wq = "pool"

    def cvtok(l, kind, f=None):
        if not cfg.preconvert:
            return []
        return [("cv", l, kind)] if f is None else [("cv", l, kind, f)]

    slot_rr = [0]

    def load_slot(parts, rtoks):
        i = slot_rr[0] % NSLOT
        slot_rr[0] += 1
        st = wslot[i]
        for tk in rtoks:
            ensure(tk)
        pump(3)
        for (dstf, src) in parts:
            P.dma(wq, dstf(st), src, reads=rtoks, writes=[("w", i)], stream="w%d" % i)
        return st, ("w", i)

    def kview(st, ncols, off=0):
        return st[:, off:off + 8 * ncols].rearrange("p (k c) -> p k c", k=8)

    def load_kcols(W_ap2d, c0, ncols, rtoks, extra=None, extra_W=None):
        parts = [(lambda st: kview(st, ncols), W_ap2d[:, c0:c0 + ncols].rearrange("(k p) c -> p k c", p=128))]
        if extra is not None:
            c1, n1 = extra
            W2 = W_ap2d if extra_W is None else extra_W
            parts.append((lambda st: kview(st, n1, off=8 * ncols),
                          W2[:, c1:c1 + n1].rearrange("(k p) c -> p k c", p=128)))
        return load_slot(parts, rtoks)

    def mm(out, lhsT, rhs, start, stop, reads, wtok):
        P.op("pe", lambda e: e.matmul(out, lhsT=lhsT, rhs=rhs, start=start, stop=stop), reads=reads, writes=[wtok])

    def rms_stats(src_fn, src_toks_fn, ncols=TM, scale=1.0 / D, all_act=False):
        bk, btok = banks[7], ("bank", 7)
        for k in range(8):
            if k % 2 == 0 or all_act:
                P.op("act", lambda e, k=k: e.activation(out=sq[:, k % 4, 0:ncols], in_=src_fn(k), func=AF.Square),
                     reads=src_toks_fn(k), writes=T("sq", k % 4))
            else:
                P.op("dve", lambda e, k=k: e.tensor_tensor(out=sq[:, k % 4, 0:ncols], in0=src_fn(k), in1=src_fn(k),
                                                           op=ALU.mult),
                     reads=src_toks_fn(k), writes=T("sq", k % 4))
            mm(bk[:, 0:ncols], onesb[:], sq[:, k % 4, 0:ncols], k == 0, k == 7, T("sq", k % 4) + T("onesb"), btok)
        P.op("act", lambda e: e.activation(out=rstd[:, 0:ncols], in_=bk[:, 0:ncols], func=AF.Ln, bias=epsb[:, 0:1], scale=scale),
             reads=[btok] + T("epsb"), writes=T("rstd"))
        P.op("act", lambda e: e.activation(out=rstd[:, 0:ncols], in_=rstd[:, 0:ncols], func=AF.Exp, scale=-0.5),
             reads=T("rstd"), writes=T("rstd"))

    def prenorm(l, n):
        rms_stats(lambda k: h[:, k, :], lambda k: T("h", k), all_act=True)
        for k in range(8):
            col = pvcol("ng", l, n, k)
            P.op("dve", lambda e, k=k, col=col: e.scalar_tensor_tensor(
                out=xn[:, k, :], in0=h[:, k, :], scalar=pv[:, col:col + 1], in1=rstd[:, :], op0=ALU.mult, op1=ALU.mult),
                reads=T("h", k) + T("rstd") + T("pv"), writes=T("xn", k))

    def postnorm_residual(l, n, half):
        rms_stats(lambda k: fT[:, k, :], lambda k: T("fT", k))
        pvt = pvh if half else pv
        ptok = T("pvh") if half else T("pv")
        def op1(k):
            col = pvcol("ng", l, n, k)
            P.op("dve", lambda e, k=k, col=col: e.scalar_tensor_tensor(
                out=fT[:, k, :], in0=fT[:, k, :], scalar=pvt[:, col:col + 1], in1=rstd[:, :], op0=ALU.mult, op1=ALU.mult),
                reads=T("fT", k) + T("rstd") + ptok, writes=T("fT", k))

        def op2(k):
            P.op("dve", lambda e, k=k: e.tensor_tensor(out=h[:, k, :], in0=h[:, k, :], in1=fT[:, k, :], op=ALU.add),
                 reads=T("fT", k) + T("h", k), writes=T("h", k))
        op1(0)
        for k in range(8):
            if k + 1 < 8:
                op1(k + 1)
            op2(k)

    def ffn(l, f):
        prenorm(l, 0 if f == 0 else 4)
        Wgu = WGU.ap()[l, f]
        Wdn = WDN.ap()[l, f]
        for grp in range(11):
            st, wt = load_kcols(Wgu, grp * 256, 256, cvtok(l, "gu", f), extra=(DFF + grp * 256, 256))
            wa = kview(st, 256)
            wb = kview(st, 256, off=2048)
            for jj in range(2):
                j = grp * 2 + jj
                pa, ta = nextbank()
                pbk, tb = nextbank()
                for k in range(8):
                    mm(pa[:, :], wa[:, k, jj * 128:(jj + 1) * 128], xn[:, k, :], k == 0, k == 7, [wt] + T("xn", k), ta)
                for k in range(8):
                    mm(pbk[:, :], wb[:, k, jj * 128:(jj + 1) * 128], xn[:, k, :], k == 0, k == 7, [wt] + T("xn", k), tb)
                P.op("act", lambda e, pa=pa, j=j: e.activation(out=sil[:, j % 2, :], in_=pa[:, :], func=AF.Silu),
                     reads=[ta], writes=T("sil", j % 2))
                P.op("dve", lambda e, pbk=pbk, j=j: e.tensor_tensor(out=hid[:, j, :], in0=sil[:, j % 2, :], in1=pbk[:, :],
                                                                  op=ALU.mult),
                     reads=[tb] + T("sil", j % 2), writes=T("hid", j))
        for mp in range(4):
            pm = [nextbank(), nextbank()]
            for half in range(2):
                parts = [(lambda st: st[:, 0:11 * 256].rearrange("p (j c) -> p j c", j=11),
                          Wdn[half * 1408:(half + 1) * 1408, mp * 256:(mp + 1) * 256].rearrange("(j p) c -> p j c", p=128))]
                st, wt = load_slot(parts, cvtok(l, "dn", f))
                wv = st[:, 0:11 * 256].rearrange("p (j c) -> p j c", j=11)
                for mi in range(2):
                    pk, tk = pm[mi]
                    for jj in range(11):
                        j = half * 11 + jj
                        mm(pk[:, :], wv[:, jj, mi * 128:(mi + 1) * 128], hid[:, j, :], j == 0, j == 21,
                           [wt] + T("hid", j), tk)
            for mi in range(2):
                m = mp * 2 + mi
                pk, tk = pm[mi]
                P.op("act", lambda e, pk=pk, m=m: e.activation(out=fT[:, m, :], in_=pk[:, :], func=AF.Copy),
                     reads=[tk], writes=T("fT", m))
        postnorm_residual(l, 1 if f == 0 else 5, True)

    def setup_kv():
        mt = h
        for k in range(8):
            P.dma("sp", h[:, k, 0:MEM], memT_d.ap()[k * 128:(k + 1) * 128, :], writes=T("h", k), stream="ld_h%d" % k)
        for l in range(L):
            rms_stats(lambda k: h[:, k, 0:MEM], lambda k: T("h", k), ncols=MEM)
            for k in range(8):
                col = pvcol("ng", l, 6, k)
                P.op("dve", lambda e, k=k, col=col: e.scalar_tensor_tensor(
                    out=xn[:, k, 0:MEM], in0=h[:, k, 0:MEM], scalar=pv[:, col:col + 1], in1=rstd[:, 0:MEM],
                    op0=ALU.mult, op1=ALU.mult), reads=T("h", k) + T("rstd") + T("pv"), writes=T("xn", k))
            Wkv = WKV.ap()[l]
            for g in range(4):
                st, wt = load_kcols(Wkv, g * 512, 512, cvtok(l, "kv"))
                wv = kview(st, 512)
                if g < 2:
                    for jj in range(4):
                        j = g * 4 + jj
                        pk, tk = nextbank()
                        for k in range(8):
                            mm(pk[:, 0:MEM], wv[:, k, jj * 128:(jj + 1) * 128], xn[:, k, 0:MEM], k == 0, k == 7,
                               [wt] + T("xn", k), tk)
                        o0 = (l * 8 + j) * 256
                        P.op("act", lambda e, pk=pk, o0=o0: e.activation(out=kmT[:, o0:o0 + 256], in_=pk[:, 0:MEM], func=AF.Copy),
                             reads=[tk], writes=T("kmT", l))
                else:
                    for mc in range(2):
                        pk, tk = nextbank()
                        for k in range(8):
                            mm(pk[:, :], xn[:, k, mc * 128:(mc + 1) * 128], wv[:, k, :], k == 0, k == 7,
                               [wt] + T("xn", k), tk)
                        o0 = (l * 2 + mc) * 1024 + (g - 2) * 512
                        P.op("act", lambda e, pk=pk, o0=o0: e.activation(out=vm[:, o0:o0 + 512], in_=pk[:, :], func=AF.Copy),
                             reads=[tk], writes=T("vm", l))

    def mixer(l):
        prenorm(l, 2)
        Win = WINB.ap()[l]
        Ct = Cst[:, l * 1028:(l + 1) * 1028].rearrange("p (h n) -> p h n", h=4)
        stg, wtg = load_kcols(Win, C_I, 8, cvtok(l, "in"))
        wg8 = kview(stg, 8)
        bk6, t6 = banks[6], ("bank", 6)
        for k in range(8):
            mm(bk6[0:4, 0:TM], wg8[:, k, 0:4], xn[:, k, :], k == 0, k == 7, [wtg] + T("xn", k), t6)
        bk7, t7 = banks[7], ("bank", 7)
        for k in range(8):
            mm(bk7[0:4, 0:TM], wg8[:, k, 4:8], xn[:, k, :], k == 0, k == 7, [wtg] + T("xn", k), t7)
        bi = bif[:, 2 * l:2 * l + 1]
        bfp = bif[:, 2 * l + 1:2 * l + 2]
        dve = lambda fn, r, w: P.op("dve", fn, reads=r, writes=w)
        act = lambda fn, r, w: P.op("act", fn, reads=r, writes=w)
        dve(lambda e: e.tensor_scalar(out=gF, in0=bk7[0:4, 0:TM], scalar1=bfp, scalar2=None, op0=ALU.add),
            [t7] + T("bif"), T("gv", 0))
        act(lambda e: e.activation(out=gT1, in_=gF, func=AF.Abs), T("gv", 0), T("gv", 0))
        act(lambda e: e.activation(out=gT2, in_=gT1, func=AF.Exp, scale=-1.0), T("gv", 0), T("gv", 1))
        act(lambda e: e.activation(out=gT2, in_=gT2, func=AF.Ln, bias=1.0), T("gv", 1), T("gv", 1))
        dve(lambda e: e.tensor_single_scalar(out=gT1, in_=gF, scalar=0.0, op=ALU.min), T("gv", 0), T("gv", 0))
        dve(lambda e: e.tensor_tensor(out=gF, in0=gT1, in1=gT2, op=ALU.subtract), T("gv", 0) + T("gv", 1), T("gv", 0))
        dve(lambda e: e.tensor_tensor_scan(out=gB, data0=ones4[:], data1=gF, initial=Bprev[:, l:l + 1],
                                           op0=ALU.mult, op1=ALU.add), T("gv", 0) + T("ones4") + [("Bprev", l)], T("gv", 1))
        dve(lambda e: e.tensor_copy(out=Bprev[:, l:l + 1], in_=gB[:, TM - 1:TM]), T("gv", 1), [("Bprev", l)])
        dve(lambda e: e.scalar_tensor_tensor(out=gI[:], in0=bk6[0:4, 0:TM], scalar=bi, in1=gB, op0=ALU.add,
                                             op1=ALU.subtract), [t6] + T("bif") + T("gv", 1), T("gI"))
        dve(lambda e: e.tensor_reduce(out=mx4[:], in_=gI[:, :].rearrange("p (c t) -> p c t", c=4), axis=AX.X, op=ALU.max),
            T("gI"), T("mx4"))
        mul = MU[:, l * 5:(l + 1) * 5]
        dve(lambda e: e.tensor_tensor_scan(out=mul[:, 1:5], data0=ones4[:, 0:4], data1=mx4[:], initial=mul[:, 0:1],
                                           op0=ALU.mult, op1=ALU.max), T("mx4") + T("ones4") + [("MU", l)], [("MU", l)])
        dve(lambda e: e.tensor_tensor(out=dec4[:], in0=mul[:, 0:4], in1=mul[:, 1:5], op=ALU.subtract), [("MU", l)], T("dec4"))
        act(lambda e: e.activation(out=dec4[:], in_=dec4[:], func=AF.Exp), T("dec4"), T("dec4"))
        mub = mul[:, 1:5].unsqueeze(2).to_broadcast([4, 4, 128])
        v3 = lambda t: t[:, :].rearrange("p (c t) -> p c t", c=4)
        dve(lambda e: e.tensor_tensor(out=v3(gT1), in0=v3(gI), in1=mub, op=ALU.subtract), T("gI") + [("MU", l)], T("gv", 0))
        dve(lambda e: e.tensor_scalar(out=gT1, in0=gT1, scalar1=-0.5 * float(np.log(128.0)), scalar2=None, op0=ALU.add),
            T("gv", 0), T("gv", 0))
        act(lambda e: e.activation(out=gW[:], in_=gT1, func=AF.Exp), T("gv", 0), T("gW"))
        dve(lambda e: e.tensor_tensor(out=v3(gT2), in0=v3(gB), in1=mub, op=ALU.add), T("gv", 1) + [("MU", l)], T("gv", 1))
        act(lambda e: e.activation(out=gE[:], in_=gT2, func=AF.Exp, scale=-2.0), T("gv", 1), T("gE"))
        dve(lambda e: e.tensor_copy(out=mul[:, 0:1], in_=mul[:, 4:5]), [("MU", l)] + T("dec4") + T("gv", 0) + T("gv", 1), [("MU", l)])
        P.op("pool", lambda e: e.memset(vaug[:, :, :, 256:257], 1.0), writes=[("vaug", c) for c in range(4)])
        tl = tails[:, l * 24:(l + 1) * 24].rearrange("p (b t) -> p b t", b=8)
        for g in range(2):
            st, wt = load_kcols(Win, C_QK + g * 512, 512, cvtok(l, "in"))
            wv = kview(st, 512)
            for jj in range(4):
                blk = g * 4 + jj
                zi = blk % 2
                pk, tk = nextbank()
                for k in range(8):
                    mm(pk[:, :], wv[:, k, jj * 128:(jj + 1) * 128], xn[:, k, :], k == 0, k == 7, [wt] + T("xn", k), tk)
                P.op("pool", lambda e, blk=blk, zi=zi: e.tensor_copy(out=zb[:, zi, 0:3], in_=tl[:, blk, :]),
                     reads=[("tails", l)], writes=T("zb", zi))
                act(lambda e, pk=pk, zi=zi: e.activation(out=zb[:, zi, 3:TM + 3], in_=pk[:, :], func=AF.Copy),
                    [tk], T("zb", zi))
                P.op("pool", lambda e, blk=blk, zi=zi: e.tensor_copy(out=tl[:, blk, :], in_=zb[:, zi, TM:TM + 3]),
                     reads=T("zb", zi), writes=[("tails", l)])
                for j in range(4):
                    col = pvcol("cw", l, j, blk)
                    if j == 0:
                        P.op("dve", lambda e, zi=zi, col=col: e.tensor_scalar(
                            out=ctmp[:, zi, :], in0=zb[:, zi, 0:TM], scalar1=pv[:, col:col + 1], scalar2=None, op0=ALU.mult),
                            reads=T("zb", zi) + T("pv"), writes=T("ctmp", zi))
                    else:
                        P.op("dve", lambda e, zi=zi, col=col, j=j: e.scalar_tensor_tensor(
                            out=ctmp[:, zi, :], in0=zb[:, zi, j:TM + j], scalar=pv[:, col:col + 1], in1=ctmp[:, zi, :],
                            op0=ALU.mult, op1=ALU.add),
                            reads=T("zb", zi) + T("pv") + T("ctmp", zi), writes=T("ctmp", zi))
                colb = pvcol("cb", l, blk)
                act(lambda e, zi=zi, blk=blk, colb=colb: e.activation(out=bufA[:, blk, :], in_=ctmp[:, zi, :], func=AF.Silu,
                                                                       bias=pv[:, colb:colb + 1]),
                    T("ctmp", zi) + T("pv"), T("bufA", blk))
        lg = rowp[:, (l * 2) * D:(l * 2 + 1) * D]
        lb = rowp[:, (l * 2 + 1) * D:(l * 2 + 2) * D]
        gv4 = gv[:, :, :].rearrange("p a (b n) -> p (a b) n", b=2)
        for g in range(2):
            st, wt = load_kcols(Win, C_SV + g * 512, 512, cvtok(l, "in"))
            wv = kview(st, 512)
            si = g % 2
            smv = sm[:, si, :]
            for c in range(4):
                pk, tk = nextbank()
                for k in range(8):
                    mm(pk[:, :], xn[:, k, c * 128:(c + 1) * 128], wv[:, k, :], k == 0, k == 7, [wt] + T("xn", k), tk)
                act(lambda e, pk=pk, c=c: e.activation(out=gv4[:, c, :], in_=pk[:, :], func=AF.Gelu_apprx_tanh),
                    [tk], T("gv", c // 2))
                for g2 in range(2):
                    q = c * 2 + g2
                    dve(lambda e, c=c, g2=g2, q=q: e.bn_stats(out=st8[:, q, :], in_=gv4[:, c, g2 * 256:(g2 + 1) * 256]),
                        T("gv", c // 2), T("st8", q))
                    dve(lambda e, q=q: e.bn_aggr(out=mv8[:, q, :], in_=st8[:, q, :]), T("st8", q), T("mv8", q))
            act(lambda e, smv=smv: e.activation(out=smv[:, 0:8], in_=mv8[:, :, 1], func=AF.Sqrt, bias=EPS),
                [("mv8", q) for q in range(8)], T("sm", si))
            dve(lambda e, smv=smv: e.reciprocal(out=smv[:, 8:16], in_=smv[:, 0:8]), T("sm", si), T("sm", si))
            c0 = g * 512
            for c in range(4):
                for g2 in range(2):
                    q = c * 2 + g2
                    dve(lambda e, g2=g2, c=c, q=q, smv=smv, c0=c0: e.tensor_scalar(
                        out=vn[:, c, c0 + g2 * 256:c0 + (g2 + 1) * 256], in0=gv4[:, c, g2 * 256:(g2 + 1) * 256],
                        scalar1=mv8[:, q, 0:1], scalar2=smv[:, 8 + q:9 + q], op0=ALU.subtract, op1=ALU.mult),
                        T("gv", c // 2) + T("mv8", q) + T("sm", si), T("vn", c))
                dve(lambda e, c=c, c0=c0: e.tensor_tensor(out=vn[:, c, c0:c0 + 512], in0=vn[:, c, c0:c0 + 512],
                                                         in1=lg[:, c0:c0 + 512], op=ALU.mult),
                    T("vn", c) + T("rowp"), T("vn", c))
                dve(lambda e, c=c, c0=c0: e.tensor_tensor(out=vn[:, c, c0:c0 + 512], in0=vn[:, c, c0:c0 + 512],
                                                         in1=lb[:, c0:c0 + 512], op=ALU.add),
                    T("vn", c) + T("rowp"), T("vn", c))
        for g in range(2):
            st, wt = load_kcols(Win, C_V + g * 512, 512, cvtok(l, "in"))
            wv = kview(st, 512)
            for c in range(4):
                pk, tk = nextbank()
                for k in range(8):
                    mm(pk[:, :], xn[:, k, c * 128:(c + 1) * 128], wv[:, k, :], k == 0, k == 7, [wt] + T("xn", k), tk)
                act(lambda e, pk=pk, c=c, g=g: e.activation(
                    out=vaug[:, c, 2 * g:2 * g + 2, 0:256], in_=pk[:, :].rearrange("p (h n) -> p h n", h=2), func=AF.Copy),
                    [tk], T("vaug", c))
        for g in range(2):
            st, wt = load_kcols(Win, C_O + g * 512, 512, cvtok(l, "in"))
            wv = kview(st, 512)
            for jj in range(4):
                blk = g * 4 + jj
                pk, tk = nextbank()
                for k in range(8):
                    mm(pk[:, :], wv[:, k, jj * 128:(jj + 1) * 128], xn[:, k, :], k == 0, k == 7, [wt] + T("xn", k), tk)
                act(lambda e, pk=pk, blk=blk: e.activation(out=bufB[:, blk, :], in_=pk[:, :], func=AF.Sigmoid),
                    [tk], T("bufB", blk))
                col = pvcol("mg", l, blk)
                P.op("dve", lambda e, blk=blk, col=col: e.tensor_scalar(
                    out=bufB[:, blk, :], in0=bufB[:, blk, :], scalar1=pv[:, col:col + 1], scalar2=None, op0=ALU.mult),
                    reads=T("bufB", blk) + T("pv"), writes=T("bufB", blk))
        for c in range(4):
            mm(bk6[:, c * 8:c * 8 + 4], gW[:, c * 128:(c + 1) * 128], sel[:, :].rearrange("p (h m) -> p h m", h=4)[:, :, 0],
               True, True, T("gW") + T("sel"), t6)
            mm(bk6[:, c * 8 + 4:c * 8 + 8], gE[:, c * 128:(c + 1) * 128], sel[:, :].rearrange("p (h m) -> p h m", h=4)[:, :, 0],
               True, True, T("gE") + T("sel"), t6)
        dve(lambda e: e.tensor_copy(out=tokW[:, :, :].rearrange("p c n -> p (c n)"), in_=bk6[:, 0:32]), [t6], T("tokW"))
        for hh in range(4):
            mm(bk7[:, hh * 4:hh * 4 + 4], sel[:, hh * 128:(hh + 1) * 128], dec4[:, :], True, True, T("dec4") + T("sel"), t7)
        dve(lambda e: e.tensor_copy(out=decb[:, :], in_=bk7[:, 0:16]), [t7], T("decb"))

        for c in range(4):
            pk, tk = nextbank()
            pkb = pk[:, :].bitcast(BF16)
            for hh in range(4):
                P.op("pe", lambda e, pkb=pkb, hh=hh, c=c: e.transpose(out=pkb[:, hh * 128:(hh + 1) * 128],
                                                                      in_=bufA[:, 4 + hh, c * 128:(c + 1) * 128],
                                                                      identity=ident[:]),
                     reads=T("bufA", 4 + hh) + T("ident"), writes=[tk])
            dve(lambda e, pkb=pkb, c=c: e.tensor_tensor(
                out=kp[:, c, :].rearrange("p (h d) -> p h d", h=4), in0=pkb[:, 0:512].rearrange("p (h d) -> p h d", h=4),
                in1=tokW[:, c, 0:4].unsqueeze(2).to_broadcast([128, 4, 128]), op=ALU.mult),
                [tk] + T("tokW"), T("kp", c))
        fill_rr = [0]

        def filler(i):
            which, g = ("u", i) if i < 2 else ("xq", i - 2)
            c0 = (C_U if which == "u" else C_XQ) + g * 512
            st, wt = load_kcols(Win, c0, 512, cvtok(l, "in"))
            wv = kview(st, 512)
            for jj in range(4):
                blk = g * 4 + jj
                bi_ = 6 + fill_rr[0] % 2
                fill_rr[0] += 1
                pk, tk = banks[bi_], ("bank", bi_)
                for k in range(8):
                    mm(pk[:, :], wv[:, k, jj * 128:(jj + 1) * 128], xn[:, k, :], k == 0, k == 7, [wt] + T("xn", k), tk)
                if which == "u":
                    act(lambda e, pk=pk, blk=blk: e.activation(out=bufC[:, blk, :], in_=pk[:, :], func=AF.Gelu_apprx_tanh),
                        [tk], T("bufC", blk))
                else:
                    act(lambda e, pk=pk, blk=blk: e.activation(out=xqT[:, blk, :], in_=pk[:, :], func=AF.Copy),
                        [tk], T("xqT", blk))

        for c in range(4):
            cs = slice(c * 128, (c + 1) * 128)
            pS, tS = nextbank()
            for hh in range(4):
                mm(pS[:, hh * 128:(hh + 1) * 128], bufA[:, 4 + hh, cs], bufA[:, hh, cs], True, True,
                   T("bufA", 4 + hh) + T("bufA", hh), tS)
            for hh in range(4):
                dve(lambda e, hh=hh, c=c, pS=pS: e.scalar_tensor_tensor(
                    out=ST[:, c, hh * 128:(hh + 1) * 128], in0=pS[:, hh * 128:(hh + 1) * 128], scalar=tokW[:, c, hh:hh + 1],
                    in1=maskb[:], op0=ALU.mult, op1=ALU.mult), [tS] + T("tokW") + T("maskb"), T("ST", c))
            for hh in range(4):
                act(lambda e, hh=hh, c=c: e.activation(out=Cbf[:, c, hh * 257:(hh + 1) * 257], in_=Ct[:, hh, :],
                                                       func=AF.Copy, scale=decb[:, hh * 4 + c:hh * 4 + c + 1]),
                    [("C", l)] + T("decb"), T("Cbf", c))
            for hh in range(4):
                pP, tP = nextbank()
                mm(pP[:, 0:257], kp[:, c, hh * 128:(hh + 1) * 128], vaug[:, c, hh, :], True, True,
                   T("kp", c) + T("vaug", c), tP)
                dve(lambda e, hh=hh, c=c, pP=pP: e.scalar_tensor_tensor(
                    out=Ct[:, hh, :], in0=Ct[:, hh, :], scalar=decb[:, hh * 4 + c:hh * 4 + c + 1], in1=pP[:, 0:257],
                    op0=ALU.mult, op1=ALU.add), [tP, ("C", l)] + T("decb"), [("C", l)])
        for c in range(4):
            cs = slice(c * 128, (c + 1) * 128)
            ci = c % 2
            si = c % 2
            smv = sm[:, si, :]
            pNs = []
            for hh in range(4):
                pN, tN = nextbank()
                pNs.append((pN, tN))
                mm(pN[:, 0:257], ST[:, c, hh * 128:(hh + 1) * 128], vaug[:, c, hh, :], True, False,
                   T("ST", c) + T("vaug", c), tN)
                mm(pN[:, 0:257], bufA[:, hh, cs], Cbf[:, c, hh * 257:(hh + 1) * 257], False, True,
                   T("bufA", hh) + T("Cbf", c), tN)
            pT8, tT8 = nextbank()
            pT8b = pT8[:, :].bitcast(BF16)
            filler(c)
            for hh in range(4):
                pN, tN = pNs[hh]
                dve(lambda e, pN=pN, hh=hh: e.bn_stats(out=st6[:, hh, :], in_=pN[:, 0:256]), [tN], T("st6", hh))
                dve(lambda e, pN=pN, hh=hh, smv=smv: e.tensor_copy(out=smv[:, hh:hh + 1], in_=pN[:, 256:257]),
                    [tN], T("sm", si))
            for hh in range(4):
                dve(lambda e, hh=hh: e.bn_aggr(out=mv[:, hh, :], in_=st6[:, hh, :]), T("st6", hh), T("mv", hh))
            dve(lambda e, smv=smv: e.tensor_tensor(out=smv[:, 4:8], in0=smv[:, 0:4], in1=smv[:, 0:4], op=ALU.mult),
                T("sm", si), T("sm", si))
            dve(lambda e, smv=smv, c=c: e.tensor_tensor(out=smv[:, 4:8], in0=smv[:, 4:8], in1=tokW[:, c, 4:8], op=ALU.max),
                T("sm", si) + T("tokW"), T("sm", si))
            dve(lambda e, smv=smv: e.scalar_tensor_tensor(out=smv[:, 8:12], in0=smv[:, 4:8], scalar=EPS, in1=mv[:, :, 1],
                                                          op0=ALU.mult, op1=ALU.add),
                T("sm", si) + [("mv", g4) for g4 in range(4)], T("sm", si))
            act(lambda e, smv=smv: e.activation(out=smv[:, 12:16], in_=smv[:, 8:12], func=AF.Sqrt), T("sm", si), T("sm", si))
            dve(lambda e, smv=smv: e.reciprocal(out=smv[:, 12:16], in_=smv[:, 12:16]), T("sm", si), T("sm", si))
            for hh in range(4):
                pN, tN = pNs[hh]
                dve(lambda e, pN=pN, hh=hh, ci=ci, smv=smv: e.tensor_scalar(
                    out=hn[:, ci, hh * 256:(hh + 1) * 256], in0=pN[:, 0:256], scalar1=mv[:, hh, 0:1],
                    scalar2=smv[:, 12 + hh:13 + hh], op0=ALU.subtract, op1=ALU.mult),
                    [tN] + T("mv", hh) + T("sm", si), T("hn", ci))
                for i2 in range(2):
                    P.op("pe", lambda e, hh=hh, i2=i2, ci=ci, pT8b=pT8b: e.transpose(
                        out=pT8b[:, (2 * hh + i2) * 128:(2 * hh + i2 + 1) * 128],
                        in_=hn[:, ci, hh * 256 + i2 * 128:hh * 256 + (i2 + 1) * 128], identity=ident[:]),
                        reads=T("hn", ci) + T("ident"), writes=[tT8])
            dve(lambda e, pT8b=pT8b, cs=cs: e.tensor_tensor(
                out=ymT[:, :, cs], in0=pT8b[:, 0:1024].rearrange("p (k t) -> p k t", k=8), in1=bufB[:, :, cs], op=ALU.mult),
                [tT8] + [("bufB", k) for k in range(8)], [("ymT", k) for k in range(8)])

        def merge_group(b, ysrc, ytok, first, last, mp):
            Wb = WBR.ap()[l, b]
            if True:
                stw, wtw = load_kcols(Wb, mp * 256, 256, cvtok(l, "br") + cvtok(l, "in"),
                                      extra=(C_G + b * 1024 + mp * 256, 256), extra_W=Win)
                wtg2 = wtw
                ww = kview(stw, 256)
                wgv = kview(stw, 256, off=2048)
                for mi in range(2):
                    m = mp * 2 + mi
                    pp, tp = nextbank()
                    pg, tg = nextbank()
                    for k in range(8):
                        mm(pg[:, :], wgv[:, k, mi * 128:(mi + 1) * 128], xn[:, k, :], k == 0, k == 7, [wtg2] + T("xn", k), tg)
                    for k in range(8):
                        mm(pp[:, :], ww[:, k, mi * 128:(mi + 1) * 128], ysrc[:, k, :], k == 0, k == 7,
                           [wtw] + [(ytok, k)], tp)
                    si = m % 2
                    act(lambda e, pg=pg, si=si: e.activation(out=sig[:, si, :], in_=pg[:, :], func=AF.Sigmoid),
                        [tg], T("sig", si))
                    if first:
                        dve(lambda e, pp=pp, si=si, m=m: e.tensor_tensor(out=fT[:, m, :], in0=pp[:, :], in1=sig[:, si, :],
                                                                         op=ALU.mult), [tp] + T("sig", si), T("fT", m))
                    else:
                        mi2 = m % 2
                        dve(lambda e, pp=pp, si=si, mi2=mi2: e.tensor_tensor(out=mtmp[:, mi2, :], in0=pp[:, :], in1=sig[:, si, :],
                                                                           op=ALU.mult), [tp] + T("sig", si), T("ctmp", mi2))
                        if not last:
                            P.op("dve", lambda e, m=m, mi2=mi2: e.tensor_tensor(out=fT[:, m, :], in0=fT[:, m, :],
                                                                                in1=mtmp[:, mi2, :], op=ALU.add),
                                 reads=T("fT", m) + T("ctmp", mi2), writes=T("fT", m))
                        else:
                            P.op("dve", lambda e, m=m, mi2=mi2: e.tensor_tensor(out=bufC[:, m, :], in0=fT[:, m, :],
                                                                                in1=mtmp[:, mi2, :], op=ALU.add),
                                 reads=T("fT", m) + T("ctmp", mi2), writes=T("bufC", m))


        for c in range(4):
            cs = slice(c * 128, (c + 1) * 128)
            pa2 = [nextbank(), nextbank()]
            for j in range(8):
                g = j // 2
                pk, tk = pa2[j // 4]
                osl = slice((j % 4) * 128, (j % 4 + 1) * 128)
                wo = (l * 4 + g) * 128
                mm(pk[:, osl], vn[:, c, j * 128:(j + 1) * 128], wsT[:, wo:wo + 128], True, False, T("vn", c) + T("wsT"), tk)
                mm(pk[:, osl], onesb[0:1, :], bshi[0:1, wo:wo + 128], False, False, T("onesb") + T("bshi"), tk)
                mm(pk[:, osl], onesb[0:1, :], bslo[0:1, wo:wo + 128], False, True, T("onesb") + T("bslo"), tk)
            for hf in range(2):
                pk, tk = pa2[hf]
                dve(lambda e, pk=pk, hf=hf, cs=cs: e.tensor_tensor(
                    out=bufB[:, 4 * hf:4 * hf + 4, cs], in0=pk[:, :].rearrange("p (k t) -> p k t", k=4),
                    in1=bufC[:, 4 * hf:4 * hf + 4, cs], op=ALU.mult),
                    [tk] + [("bufC", k) for k in range(4 * hf, 4 * hf + 4)], [("bufB", k) for k in range(4 * hf, 4 * hf + 4)])
        def m5_s1(c):
            cs = slice(c * 128, (c + 1) * 128)
            pi = c % 2
            psc = [nextbank(), nextbank()]
            for hh in range(4):
                pk, tk = psc[hh // 2]
                osl = slice((hh % 2) * 256, (hh % 2 + 1) * 256)
                for i2 in range(2):
                    j = 2 * hh + i2
                    o0 = (l * 8 + j) * 256
                    mm(pk[:, osl], xqT[:, j, cs], kmT[:, o0:o0 + 256], i2 == 0, i2 == 1, T("xqT", j) + T("kmT", l), tk)
            si = c % 2
            smv = sm[:, si, :]
            for hf in range(2):
                pk, tk = psc[hf]
                dve(lambda e, pk=pk, hf=hf, smv=smv: e.tensor_reduce(
                    out=smv[:, 2 * hf:2 * hf + 2], in_=pk[:, :].rearrange("p (h m) -> p h m", h=2), axis=AX.X, op=ALU.max),
                    [tk], T("sm", si))
            dve(lambda e, smv=smv: e.tensor_scalar(out=smv[:, 4:8], in0=smv[:, 0:4], scalar1=-1.0 / 16.0, scalar2=None,
                                                   op0=ALU.mult), T("sm", si), T("sm", si))
            for hh in range(4):
                pk, tk = psc[hh // 2]
                osl = slice((hh % 2) * 256, (hh % 2 + 1) * 256)
                act(lambda e, pk=pk, osl=osl, hh=hh, pi=pi, smv=smv: e.activation(
                    out=pb[:, pi, hh * 256:(hh + 1) * 256], in_=pk[:, osl], func=AF.Exp, bias=smv[:, 4 + hh:5 + hh],
                    scale=1.0 / 16.0, accum_out=smv[:, 8 + hh:9 + hh]), [tk] + T("sm", si), T("hn", pi) + T("sm", si))
            dve(lambda e, smv=smv: e.reciprocal(out=smv[:, 12:16], in_=smv[:, 8:12]), T("sm", si), T("sm", si))
            dve(lambda e, pi=pi, smv=smv: e.tensor_tensor(
                out=pb[:, pi, :].rearrange("p (h m) -> p h m", h=4), in0=pb[:, pi, :].rearrange("p (h m) -> p h m", h=4),
                in1=smv[:, 12:16].unsqueeze(2).to_broadcast([128, 4, 256]), op=ALU.mult),
                T("hn", pi) + T("sm", si), T("hn", pi))
        def m5_s2(c):
            cs = slice(c * 128, (c + 1) * 128)
            pi = c % 2
            pk8, tk8 = nextbank()
            pk8b = pk8[:, :].bitcast(BF16)
            for hh in range(4):
                for mc in range(2):
                    q8 = hh * 2 + mc
                    P.op("pe", lambda e, pk8b=pk8b, q8=q8, pi=pi, hh=hh, mc=mc: e.transpose(
                        out=pk8b[:, q8 * 128:(q8 + 1) * 128], in_=pb[:, pi, hh * 256 + mc * 128:hh * 256 + (mc + 1) * 128],
                        identity=ident[:]), reads=T("hn", pi) + T("ident"), writes=[tk8])
            act(lambda e, pk8b=pk8b, pi=pi: e.activation(out=pT[:, pi, :], in_=pk8b[:, 0:1024], func=AF.Copy),
                [tk8], T("pT", pi))
            pyx = [nextbank(), nextbank()]
            for j in range(8):
                hh = j // 2
                pk, tk = pyx[j // 4]
                osl = slice((j % 4) * 128, (j % 4 + 1) * 128)
                for mc in range(2):
                    o0 = (l * 2 + mc) * 1024 + j * 128
                    q8 = hh * 2 + mc
                    mm(pk[:, osl], vm[:, o0:o0 + 128], pT[:, pi, q8 * 128:(q8 + 1) * 128], mc == 0, mc == 1,
                       T("vm", l) + T("pT", pi), tk)
            for hf in range(2):
                pk, tk = pyx[hf]
                act(lambda e, pk=pk, hf=hf, cs=cs: e.activation(
                    out=bufA[:, 4 * hf:4 * hf + 4, cs], in_=pk[:, :].rearrange("p (k t) -> p k t", k=4), func=AF.Copy),
                    [tk], [("bufA", k) for k in range(4 * hf, 4 * hf + 4)])

        fence()
        for c in range(4):
            m5_s1(c)
            merge_group(0, ymT, "ymT", True, False, c)
            merge_group(1, bufB, "bufB", False, False, c)
            m5_s2(c)
        for mp in range(4):
            merge_group(2, bufA, "bufA", False, True, mp)
        Wo = WOUT.ap()[l]
        for g in range(2):
            st, wt = load_kcols(Wo, g * 512, 512, cvtok(l, "out"))
            wv = kview(st, 512)
            for jj in range(4):
                m = g * 4 + jj
                pk, tk = nextbank()
                for k in range(8):
                    mm(pk[:, :], wv[:, k, jj * 128:(jj + 1) * 128], bufC[:, k, :], k == 0, k == 7, [wt] + T("bufC", k), tk)
                act(lambda e, pk=pk, m=m: e.activation(out=fT[:, m, :], in_=pk[:, :], func=AF.Copy), [tk], T("fT", m))
        postnorm_residual(l, 3, False)

    setup_kv()
    for t in range(NT):
        ts = slice(t * TM, (t + 1) * TM)
        for k in range(8):
            P.dma("sp", h[:, k, :], xT_d.ap()[k * 128:(k + 1) * 128, ts], writes=T("h", k), stream="ld_h%d" % k)
        for l in range(L):
            ffn(l, 0)
            if cfg.stages == "ffn1":
                continue
            fence()
            mixer(l)
            fence()
            if cfg.stages == "mix":
                continue
            ffn(l, 1)
        for k in range(8):
            P.dma("sp", yT_d.ap()[k * 128:(k + 1) * 128, ts], h[:, k, :], reads=T("h", k), writes=[("yT", k)], stream="st_y%d" % k)
    pump(len(jobs))
    P.barrier("sp", [("yT", k) for k in range(8)])
    P.emit()
    return nc, P


def prep_shared(inp, cfg):
    L = cfg.L
    f = lambda a: np.ascontiguousarray(np.asarray(a, dtype=np.float32))
    ng = f(inp["norm_g"])[:L]
    cw = f(inp["conv_w"])[:L]
    cb = f(inp["conv_b"])[:L]
    mg = f(inp["mlstm_norm_g"])[:L]
    pv = np.concatenate([
        ng.reshape(L, 7, 8, 128).transpose(3, 0, 1, 2).reshape(128, L * 56),
        cw.reshape(L, 4, 8, 128).transpose(3, 0, 1, 2).reshape(128, L * 32),
        cb.reshape(L, 8, 128).transpose(2, 0, 1).reshape(128, L * 8),
        mg.reshape(L, 8, 128).transpose(2, 0, 1).reshape(128, L * 8)], axis=1)
    lg = f(inp["sgu_ln_g"])[:L]
    lb = f(inp["sgu_ln_b"])[:L]
    rowp = np.stack([lg, lb], axis=1).reshape(1, L * 2 * D)
    rowp = np.ascontiguousarray(np.broadcast_to(rowp, (128, L * 2 * D)))
    ws = f(inp["sgu_w_s"])[:L]
    wsT = np.ascontiguousarray(ws.transpose(3, 0, 1, 2).reshape(128, L * 4 * 128))
    bs = f(inp["sgu_b_s"])[:L].reshape(1, L * 512)
    bif = np.stack([f(inp["b_i"])[:L], f(inp["b_f"])[:L]], axis=1)
    bif = np.ascontiguousarray(bif.transpose(2, 0, 1).reshape(4, 2 * L))
    ident = np.eye(128, dtype=np.float32).astype(ml_dtypes.bfloat16)
    maskle = np.triu(np.ones((128, 128), dtype=np.float32)).astype(ml_dtypes.bfloat16)
    sel = np.zeros((4, 4, 128), dtype=np.float32)
    for hh in range(4):
        sel[hh, hh, :] = 1.0
    sel = sel.reshape(4, 512)
    return {
        "w_gu": f(inp["w_ffn_gu"])[:L], "w_dn": f(inp["w_ffn_down"])[:L], "w_in": f(inp["w_in"])[:L],
        "w_kv": f(inp["w_mkv"])[:L], "w_br": f(inp["w_branch"])[:L], "w_out": f(inp["w_out"])[:L],
        "pvec": np.ascontiguousarray(pv), "rowp": rowp, "wsT": wsT, "bs": np.ascontiguousarray(bs), "bif": bif,
        "ident": ident, "maskle": maskle, "sel": sel,
    }


_CACHE = {}


def run(inp, cfg, n_cores, trace=False):
    key = (cfg.S, cfg.L, cfg.stages, cfg.preconvert)
    if key not in _CACHE:
        _CACHE[key] = build_program(cfg)
    nc, P = _CACHE[key]
    shared = prep_shared(inp, cfg)
    x = np.asarray(inp["x"], dtype=np.float32)
    mem = np.asarray(inp["mem"], dtype=np.float32)
    in_maps = []
    for b in range(n_cores):
        m = dict(shared)
        m["xT"] = np.ascontiguousarray(x[b, :cfg.S].T)
        m["memT"] = np.ascontiguousarray(mem[b].T)
        in_maps.append(m)
    res = run_bass_kernel_spmd(nc, in_maps, core_ids=list(range(n_cores)), trace=trace)
    out = np.stack([np.ascontiguousarray(r["yT"].T) for r in res.results], axis=0)
    return out.astype(np.float32), res


def kernel(x, mem, norm_g, w_ffn_gu, w_ffn_down, w_in, conv_w, conv_b, b_i, b_f, mlstm_norm_g, sgu_ln_g, sgu_ln_b,
           sgu_w_s, sgu_b_s, w_mkv, w_branch, w_out):
    inp = dict(x=x, mem=mem, norm_g=norm_g, w_ffn_gu=w_ffn_gu, w_ffn_down=w_ffn_down, w_in=w_in, conv_w=conv_w,
               conv_b=conv_b, b_i=b_i, b_f=b_f, mlstm_norm_g=mlstm_norm_g, sgu_ln_g=sgu_ln_g, sgu_ln_b=sgu_ln_b,
               sgu_w_s=sgu_w_s, sgu_b_s=sgu_b_s, w_mkv=w_mkv, w_branch=w_branch, w_out=w_out)
    cfg = Cfg(S=4096, L=2, stages="all", preconvert=True)
    out, _ = run(inp, cfg, 8)
    return out
```
